# Optimizing a Trainium2 kernel written in Bass

```python
import jax, jax.numpy as jnp
from jax import lax
import numpy as np

D_MODEL = 2048
BATCH = 4
SEQ = 2048
DEPTH = 2

MEM_LEN = 256
GRID_W = 64
ROPE_THETA = 10000.0
EPS = 1e-6

N_BRANCH = 4
BRANCH_W = D_MODEL // 4

NA_HEADS = 4
NA_HD = BRANCH_W // NA_HEADS
NA_WIN_R = 8
NA_WIN_C = 16

MLA_HEADS = 4
MLA_NOPE = 128
MLA_ROPE = 64
MLA_V = BRANCH_W // MLA_HEADS
MLA_Q_RANK = (3 * D_MODEL) // 8
MLA_KV_RANK = D_MODEL // 8

SWA_HEADS = 8
SWA_KV_HEADS = 2
SWA_HD = BRANCH_W // SWA_HEADS
SWA_WINDOW = 128
SWA_BLOCK = 128

MEM_HEADS = 4
MEM_HD = BRANCH_W // MEM_HEADS

Q_BLOCK = 128

D_FF = 5632
N_EXPERTS = 8
TOP_K = 2
D_FF_EXPERT = 7168
N_DENSE = (DEPTH + 1) // 2
N_MOE = DEPTH // 2

SPLIT_SIZES = [NA_HEADS * NA_HD, NA_HEADS * NA_HD, NA_HEADS * NA_HD,
               MLA_Q_RANK, MLA_KV_RANK, MLA_ROPE,
               SWA_HEADS * SWA_HD, SWA_KV_HEADS * SWA_HD, SWA_KV_HEADS * SWA_HD,
               MEM_HEADS * MEM_HD]
IN_COLS = int(sum(SPLIT_SIZES))
SPLIT_POINTS = [int(c) for c in np.cumsum(SPLIT_SIZES)[:-1]]

kernel_name = "hybrid_gated_parallel_mixer_encoder"


def rmsnorm(x, g):
    xf = x.astype(jnp.float32)
    y = xf * lax.rsqrt(jnp.mean(xf * xf, axis=-1, keepdims=True) + EPS)
    return (y * g.astype(jnp.float32)).astype(x.dtype)


def rope(x, pos):
    d = x.shape[-1]
    half = d // 2
    freqs = ROPE_THETA ** (-2.0 * jnp.arange(half, dtype=jnp.float32) / d)
    ang = pos.astype(jnp.float32)[:, None] * freqs[None, :]
    cos = jnp.cos(ang)[:, None, :]
    sin = jnp.sin(ang)[:, None, :]
    xf = x.astype(jnp.float32)
    x1, x2 = xf[..., :half], xf[..., half:]
    return jnp.concatenate([x1 * cos - x2 * sin, x2 * cos + x1 * sin], axis=-1).astype(x.dtype)


def neighbourhood_attention(q, k, v, rpb):
    B, S, H, D = q.shape
    rows = S // GRID_W
    wr = min(NA_WIN_R, rows)
    qg = q.reshape(B, rows, GRID_W, H, D)
    kg = k.reshape(B, rows, GRID_W, H, D)
    vg = v.reshape(B, rows, GRID_W, H, D)
    col = jnp.arange(GRID_W)
    col_start = jnp.clip(col - NA_WIN_C // 2, 0, GRID_W - NA_WIN_C)
    col_idx = col_start[:, None] + jnp.arange(NA_WIN_C)[None, :]
    dc = col_idx - col[:, None] + (NA_WIN_C - 1)
    scale = D ** -0.5

    def row_block(args):
        r, q_r = args
        rs = jnp.clip(r - wr // 2, 0, rows - wr)
        k_rows = lax.dynamic_slice_in_dim(kg, rs, wr, axis=1)
        v_rows = lax.dynamic_slice_in_dim(vg, rs, wr, axis=1)
        k_nb = k_rows[:, :, col_idx]
        v_nb = v_rows[:, :, col_idx]
        s = jnp.einsum('bqhd,biqjhd->bhqij', q_r, k_nb).astype(jnp.float32) * scale
        dr = rs + jnp.arange(wr) - r + (NA_WIN_R - 1)
        bias = rpb[:, dr][:, :, dc].astype(jnp.float32)
        s = s + jnp.transpose(bias, (0, 2, 1, 3))[None]
        p = jax.nn.softmax(s.reshape(B, H, GRID_W, wr * NA_WIN_C), axis=-1)
        p = p.reshape(B, H, GRID_W, wr, NA_WIN_C).astype(v.dtype)
        return jnp.einsum('bhqij,biqjhd->bqhd', p, v_nb)

    o = lax.map(row_block, (jnp.arange(rows), jnp.transpose(qg, (1, 0, 2, 3, 4))))
    return jnp.transpose(o, (1, 0, 2, 3, 4)).reshape(B, S, H * D)


def dense_block_attention(q, k, v, scale):
    B, S, H, Dq = q.shape
    Dv = v.shape[-1]
    nb = S // Q_BLOCK
    qb = jnp.transpose(q.reshape(B, nb, Q_BLOCK, H, Dq), (1, 0, 2, 3, 4))

    def one(q_blk):
        s = jnp.einsum('bqhd,bkhd->bhqk', q_blk, k).astype(jnp.float32) * scale
        p = jax.nn.softmax(s, axis=-1).astype(v.dtype)
        return jnp.einsum('bhqk,bkhd->bqhd', p, v)

    o = lax.map(one, qb)
    return jnp.transpose(o, (1, 0, 2, 3, 4)).reshape(B, S, H * Dv)


def mla_branch(c_q, c_kv, k_pe, pos, g_cq, w_uq, g_ckv, w_ukv, g_qn, g_kn):
    B, S, _ = c_q.shape
    q = (rmsnorm(c_q, g_cq) @ w_uq).reshape(B, S, MLA_HEADS, MLA_NOPE + MLA_ROPE)
    kv = (rmsnorm(c_kv, g_ckv) @ w_ukv).reshape(B, S, MLA_HEADS, MLA_NOPE + MLA_V)
    k_nope, v = kv[..., :MLA_NOPE], kv[..., MLA_NOPE:]
    k_rope = jnp.broadcast_to(k_pe[:, :, None, :], (B, S, MLA_HEADS, MLA_ROPE))
    k = jnp.concatenate([k_nope, k_rope], axis=-1)
    q = rmsnorm(q, g_qn)
    k = rmsnorm(k, g_kn)
    q = jnp.concatenate([q[..., :MLA_NOPE], rope(q[..., MLA_NOPE:], pos)], axis=-1)
    k = jnp.concatenate([k[..., :MLA_NOPE], rope(k[..., MLA_NOPE:], pos)], axis=-1)
    return dense_block_attention(q, k, v, (MLA_NOPE + MLA_ROPE) ** -0.5)


def sliding_window_gqa(q, k, v, sink):
    B, S, Hq, D = q.shape
    Hkv = k.shape[2]
    G = Hq // Hkv
    nb = S // SWA_BLOCK
    padw = ((0, 0), (SWA_BLOCK, SWA_BLOCK), (0, 0), (0, 0))
    kb = jnp.pad(k, padw).reshape(B, nb + 2, SWA_BLOCK, Hkv, D)
    vb = jnp.pad(v, padw).reshape(B, nb + 2, SWA_BLOCK, Hkv, D)
    k_band = jnp.concatenate([kb[:, :-2], kb[:, 1:-1], kb[:, 2:]], axis=2)
    v_band = jnp.concatenate([vb[:, :-2], vb[:, 1:-1], vb[:, 2:]], axis=2)
    qb = q.reshape(B, nb, SWA_BLOCK, Hkv, G, D)
    s = jnp.einsum('bnqhgd,bnkhd->bnhgqk', qb, k_band).astype(jnp.float32) * (D ** -0.5)
    qpos = jnp.arange(nb)[:, None] * SWA_BLOCK + jnp.arange(SWA_BLOCK)[None, :]
    kpos = jnp.arange(nb)[:, None] * SWA_BLOCK - SWA_BLOCK + jnp.arange(3 * SWA_BLOCK)[None, :]
    valid = ((jnp.abs(qpos[:, :, None] - kpos[:, None, :]) <= SWA_WINDOW)
             & (kpos >= 0)[:, None, :] & (kpos < S)[:, None, :])
    s = jnp.where(valid[None, :, None, None], s, -1e30)
    sink_b = jnp.broadcast_to(sink.astype(jnp.float32).reshape(1, 1, Hkv, G, 1, 1),
                              s.shape[:-1] + (1,))
    p = jax.nn.softmax(jnp.concatenate([s, sink_b], axis=-1), axis=-1)[..., :-1]
    o = jnp.einsum('bnhgqk,bnkhd->bnqhgd', p.astype(v.dtype), v_band)
    return o.reshape(B, S, Hq * D)


def memory_attention(q, k, v):
    B, S, H, D = q.shape
    s = jnp.einsum('bshd,bmhd->bhsm', q, k).astype(jnp.float32) * (D ** -0.5)
    p = jax.nn.softmax(s, axis=-1).astype(v.dtype)
    return jnp.einsum('bhsm,bmhd->bshd', p, v).reshape(B, S, H * D)


def swiglu(x, w_up, w_down):
    g, u = jnp.split(x @ w_up, 2, axis=-1)
    return (jax.nn.silu(g) * u) @ w_down


def moe_swiglu(x, w_router, w_up, w_down):
    logits = (x @ w_router).astype(jnp.float32)
    top_v, top_i = lax.top_k(logits, TOP_K)
    top_w = jax.nn.softmax(top_v, axis=-1)
    combine = jnp.sum(jax.nn.one_hot(top_i, N_EXPERTS, dtype=jnp.float32) * top_w[..., None], axis=-2)
    combine = combine.astype(x.dtype)
    out = jnp.zeros_like(x)
    for e in range(N_EXPERTS):
        out = out + combine[..., e:e + 1] * swiglu(x, w_up[e], w_down[e])
    return out


def setup_inputs(seed: int = 0) -> dict:
    key = jax.random.key(seed)
    ks = iter(jax.random.split(key, 40))
    f32 = jnp.float32

    def normal(shape, scale):
        return jax.random.normal(next(ks), shape, f32) * scale

    def gain(shape):
        return 1.0 + normal(shape, 0.02)

    D = D_MODEL
    return {
        "x": normal((BATCH, SEQ, D), 1.0),
        "mem": normal((BATCH, MEM_LEN, D), 1.0),
        "norm_mix": gain((DEPTH, D)),
        "w_in": normal((DEPTH, D, IN_COLS), D ** -0.5),
        "na_q_norm": gain((DEPTH, NA_HD)),
        "na_k_norm": gain((DEPTH, NA_HD)),
        "na_rpb": normal((DEPTH, NA_HEADS, 2 * NA_WIN_R - 1, 2 * NA_WIN_C - 1), 0.1),
        "mla_cq_norm": gain((DEPTH, MLA_Q_RANK)),
        "mla_w_uq": normal((DEPTH, MLA_Q_RANK, MLA_HEADS * (MLA_NOPE + MLA_ROPE)), MLA_Q_RANK ** -0.5),
        "mla_ckv_norm": gain((DEPTH, MLA_KV_RANK)),
        "mla_w_ukv": normal((DEPTH, MLA_KV_RANK, MLA_HEADS * (MLA_NOPE + MLA_V)), MLA_KV_RANK ** -0.5),
        "mla_q_norm": gain((DEPTH, MLA_NOPE + MLA_ROPE)),
        "mla_k_norm": gain((DEPTH, MLA_NOPE + MLA_ROPE)),
        "swa_q_norm": gain((DEPTH, SWA_HD)),
        "swa_k_norm": gain((DEPTH, SWA_HD)),
        "swa_sink": normal((DEPTH, SWA_HEADS), 0.5),
        "mem_norm": gain((DEPTH, D)),
        "mem_w_kv": normal((DEPTH, D, 2 * MEM_HEADS * MEM_HD), D ** -0.5),
        "mem_q_norm": gain((DEPTH, MEM_HD)),
        "mem_k_norm": gain((DEPTH, MEM_HD)),
        "w_branch": normal((DEPTH, N_BRANCH, BRANCH_W, D), BRANCH_W ** -0.5),
        "w_gate": normal((DEPTH, D, N_BRANCH, D), D ** -0.5),
        "b_gate": normal((DEPTH, N_BRANCH, D), 0.1),
        "w_o": normal((DEPTH, D, D), (N_BRANCH * D) ** -0.5),
        "norm_ffn": gain((DEPTH, D)),
        "ffn_w_up": normal((N_DENSE, D, 2 * D_FF), D ** -0.5),
        "ffn_w_down": normal((N_DENSE, D_FF, D), D_FF ** -0.5),
        "moe_router": normal((N_MOE, D, N_EXPERTS), D ** -0.5),
        "moe_w_up": normal((N_MOE, N_EXPERTS, D, 2 * D_FF_EXPERT), D ** -0.5),
        "moe_w_down": normal((N_MOE, N_EXPERTS, D_FF_EXPERT, D), D_FF_EXPERT ** -0.5),
    }


def reference(x, mem, norm_mix, w_in, na_q_norm, na_k_norm, na_rpb,
              mla_cq_norm, mla_w_uq, mla_ckv_norm, mla_w_ukv, mla_q_norm, mla_k_norm,
              swa_q_norm, swa_k_norm, swa_sink,
              mem_norm, mem_w_kv, mem_q_norm, mem_k_norm,
              w_branch, w_gate, b_gate, w_o, norm_ffn,
              ffn_w_up, ffn_w_down, moe_router, moe_w_up, moe_w_down):
    B, S, _ = x.shape
    M = mem.shape[1]
    pos = jnp.arange(S)
    h = x
    for l in range(DEPTH):
        u = rmsnorm(h, norm_mix[l])
        z = u @ w_in[l]
        (na_q, na_k, na_v, c_q, c_kv, k_pe,
         sw_q, sw_k, sw_v, me_q) = jnp.split(z, SPLIT_POINTS, axis=-1)

        qa = rmsnorm(na_q.reshape(B, S, NA_HEADS, NA_HD), na_q_norm[l])
        ka = rmsnorm(na_k.reshape(B, S, NA_HEADS, NA_HD), na_k_norm[l])
        va = na_v.reshape(B, S, NA_HEADS, NA_HD)
        o_na = neighbourhood_attention(qa, ka, va, na_rpb[l])

        o_mla = mla_branch(c_q, c_kv, k_pe, pos, mla_cq_norm[l], mla_w_uq[l],
                           mla_ckv_norm[l], mla_w_ukv[l], mla_q_norm[l], mla_k_norm[l])

        qc = rope(rmsnorm(sw_q.reshape(B, S, SWA_HEADS, SWA_HD), swa_q_norm[l]), pos)
        kc = rope(rmsnorm(sw_k.reshape(B, S, SWA_KV_HEADS, SWA_HD), swa_k_norm[l]), pos)
        vc = sw_v.reshape(B, S, SWA_KV_HEADS, SWA_HD)
        o_swa = sliding_window_gqa(qc, kc, vc, swa_sink[l])

        mkv = (rmsnorm(mem, mem_norm[l]) @ mem_w_kv[l]).reshape(B, M, 2, MEM_HEADS, MEM_HD)
        mk = rmsnorm(mkv[:, :, 0], mem_k_norm[l])
        mv = mkv[:, :, 1]
        mq = rmsnorm(me_q.reshape(B, S, MEM_HEADS, MEM_HD), mem_q_norm[l])
        o_mem = memory_attention(mq, mk, mv)

        br = jnp.stack([o_na, o_mla, o_swa, o_mem], axis=2)
        proj = jnp.einsum('bsnc,ncd->bsnd', br, w_branch[l])
        gate_logits = jnp.einsum('bsd,dne->bsne', u, w_gate[l]) + b_gate[l]
        gates = jax.nn.sigmoid(gate_logits.astype(jnp.float32)).astype(proj.dtype)
        merged = jnp.einsum('bsnd,bsnd->bsd', gates, proj)
        h = h + merged @ w_o[l]

        hn = rmsnorm(h, norm_ffn[l])
        if l % 2 == 0:
            h = h + swiglu(hn, ffn_w_up[l // 2], ffn_w_down[l // 2])
        else:
            h = h + moe_swiglu(hn, moe_router[l // 2], moe_w_up[l // 2], moe_w_down[l // 2])
    return h
```

```python
import contextlib
import numpy as np
import concourse.bass as bass
import concourse.mybir as mybir
from concourse.bass_utils import run_bass_kernel_spmd

F32 = mybir.dt.float32
BF16 = mybir.dt.bfloat16
AF = mybir.ActivationFunctionType
ALU = mybir.AluOpType
AX = mybir.AxisListType

D = 2048
EPS = 1e-6
NEG = -30000.0
FUSED = True
DEBUG = False
STOP = None
CLEAR_PROTO = False
USE_INTERNAL = True
STAGES = ["U", "NA", "MLA", "SWA", "MEM", "MERGE", "FFN"]


class Buf:
    __slots__ = ("name", "w", "r", "dsem", "gen")

    def __init__(self, name=""):
        self.name = name
        self.w = None
        self.r = []
        self.dsem = None
        self.gen = -1


class Sched:
    ENGS = ("pe", "act", "dve", "pool", "sp")

    def __init__(self, nc):
        self.nc = nc
        self.ops = {e: [] for e in self.ENGS}
        self.nops = {e: 0 for e in self.ENGS}
        self.seen = {e: {} for e in self.ENGS}
        self.signal = {e: set() for e in self.ENGS}
        self.n_sem = {"s": 0, "p": 0}
        self.sem_total = {"s": [], "p": []}
        self.next_free = {"s": 0, "p": 0}
        self.gen = 0
        self.pending_dma = []

    def _need(self, eng, tok, waits):
        if tok is None:
            return
        if tok[0] == "c":
            _, te, idx = tok
            if te == eng and eng in ("pe", "sp"):
                return
            key = ("c", te)
            if self.seen[eng].get(key, -1) >= idx:
                return
            self.seen[eng][key] = idx
            waits.append(tok)
            self.signal[te].add(idx)
        else:
            _, sem, val, g = tok
            key = ("d", sem)
            if self.seen[eng].get(key, -1) >= val:
                return
            self.seen[eng][key] = val
            waits.append(tok)

    def _deps(self, eng, reads, writes):
        waits = []
        for b in reads:
            self._need(eng, b.w, waits)
        for b in writes:
            self._need(eng, b.w, waits)
            for t in b.r:
                self._need(eng, t, waits)
        return waits

    def op(self, eng, fn, reads=(), writes=()):
        waits = self._deps(eng, reads, writes)
        idx = self.nops[eng]
        self.nops[eng] += 1
        tok = ("c", eng, idx)
        self.ops[eng].append(("op", fn, waits, idx))
        for b in reads:
            b.r.append(tok)
        for b in writes:
            b.w = tok
            b.r = []
        return tok

    def dma(self, eng, fn, reads=(), writes=()):
        waits = self._deps(eng, reads, writes)
        db = writes[0]
        cl = "p" if eng == "pool" else "s"
        if db.dsem is None or db.gen != self.gen or db.dsem[0] != cl:
            if self.next_free[cl] >= self.n_sem[cl]:
                self.n_sem[cl] += 1
                self.sem_total[cl].append(0)
            db.dsem = (cl, self.next_free[cl])
            self.next_free[cl] += 1
            db.gen = self.gen
        self.sem_total[cl][db.dsem[1]] += 16
        tok = ("d", db.dsem, self.sem_total[cl][db.dsem[1]], self.gen)
        self.ops[eng].append(("dma", fn, waits, db.dsem))
        self.pending_dma.append(tok)
        for b in reads:
            b.r.append(tok)
        for b in writes:
            b.w = tok
            b.r = []
        return tok

    def barrier(self):
        last = {}
        for e in self.ENGS:
            if self.nops[e] > 0:
                last[e] = ("c", e, self.nops[e] - 1)
        dm = {}
        for t in self.pending_dma:
            dm[t[1]] = max(dm.get(t[1], 0), t[2])
        self.pending_dma = []
        for e in self.ENGS:
            waits = []
            for te, tok in last.items():
                if te != e or e not in ("pe", "sp"):
                    self._need(e, tok, waits)
            for sem, val in dm.items():
                self._need(e, ("d", sem, val, self.gen), waits)
            self.ops[e].append(("wait", None, waits, None))
        npool = self.next_free["p"]
        if npool > 0 and CLEAR_PROTO:
            toks = [self.op(e, I("nop")) for e in ("pe", "act", "dve", "sp")]
            waits = []
            for t in toks:
                self._need("pool", t, waits)
            self.ops["pool"].append(("clear", None, waits, npool))
            tp = self.op("pool", I("nop"))
            for e in ("pe", "act", "dve", "sp"):
                waits = []
                self._need(e, tp, waits)
                self.ops[e].append(("wait", None, waits, None))
            for i in range(npool):
                self.sem_total["p"][i] = 0
            for e in self.ENGS:
                for i in range(npool):
                    self.seen[e].pop(("d", ("p", i)), None)
        self.gen += 1
        self.next_free = {"s": 0, "p": 0}

    def emit(self):
        nc = self.nc
        stack = contextlib.ExitStack()
        with stack:
            csem = {e: stack.enter_context(nc.semaphore("cs_" + e)) for e in self.ENGS}
            dsem = {(cl, i): stack.enter_context(nc.semaphore("ds%s%d" % (cl, i))) for cl in ("s", "p") for i in range(self.n_sem[cl])}
            print("semaphores: sp-dma %d pool-dma %d" % (self.n_sem["s"], self.n_sem["p"]))
            rank = {}
            for e in self.ENGS:
                for r, idx in enumerate(sorted(self.signal[e])):
                    rank[(e, idx)] = r + 1
            block = stack.enter_context(nc.Block())
            handles = {"pe": block.tensor, "act": block.scalar, "dve": block.vector,
                       "pool": block.gpsimd, "sp": block.sync}

            used_p = set()

            def make(e):
                def body(eng):
                    for kind, fn, waits, extra in self.ops[e]:
                        for t in waits:
                            if t[0] == "c":
                                eng.wait_ge(csem[t[1]], rank[(t[1], t[2])])
                            else:
                                eng.wait_ge(dsem[t[1]], t[2])
                        if kind == "op":
                            ins = getattr(eng, fn[0])(*fn[1], **fn[2])
                            if (e, extra) in rank:
                                ins.then_inc(csem[e], 1)
                        elif kind == "clear":
                            if CLEAR_PROTO != "nop":
                                for i in range(extra):
                                    eng.sem_clear(dsem[("p", i)])
                        elif kind == "dma":
                            getattr(eng, fn[0])(*fn[1], **fn[2]).then_inc(dsem[extra], 16)
                return body

            for e in self.ENGS:
                handles[e](make(e))


class Arena:
    def __init__(self, t, nbytes):
        self.t = t
        self.n = nbytes
        self.top = 0
        self.peak = 0

    def alloc(self, shape, dt):
        es = 4 if dt == F32 else 2
        n = int(np.prod(shape))
        nb = (n * es + 31) // 32 * 32
        off = self.top
        self.top += nb
        self.peak = max(self.peak, self.top)
        assert self.top <= self.n, "arena overflow %d > %d" % (self.top, self.n)
        v = self.t[:, off // 2: off // 2 + (n * es) // 2]
        if dt == F32:
            v = v.bitcast(F32)
        if len(shape) == 2:
            v = v.rearrange("p (a b) -> p a b", a=shape[0])
        elif len(shape) == 3:
            v = v.rearrange("p (a b c) -> p a b c", a=shape[0], b=shape[1])
        elif len(shape) == 4:
            v = v.rearrange("p (a b c d) -> p a b c d", a=shape[0], b=shape[1], c=shape[2])
        return v

    def mark(self):
        return self.top

    def release(self, m):
        self.top = m


def I(name, *args, **kw):
    return (name, args, kw)


def bcast(ap, shape):
    return ap.unsqueeze(len(ap.shape)).to_broadcast(list(shape))


def bc_mid(ap, n):
    a = [list(x) for x in ap.ap]
    return bass.AP(ap.tensor, ap.offset, [a[0], [0, n]] + a[1:])


GSEC = {}
_o = 0
for _n, _s in [("mix", 2048), ("naq", 512), ("nak", 512), ("cq", 768), ("ckv", 256), ("mq", 768),
               ("mkn", 512), ("mkr", 64), ("sq", 512), ("sk", 128), ("memn", 2048), ("memq", 512),
               ("memk", 512), ("ffn", 2048), ("sink", 64)]:
    GSEC[_n] = (_o, _s)
    _o += _s
NGV = _o


def build(passes, layers, use_moe):
    nc = bass.Bass("TRN2", target_bir_lowering=False)

    def din(name, shape):
        return nc.dram_tensor(name, list(shape), F32, kind="ExternalInput").ap()

    xin = din("xin", [16, 128, D])
    memd = din("mem", [2, 128, D])
    csd = din("cs", [128, 16, 64])
    lseld = din("lsel", [128, 2])
    identd = din("ident", [128, 128])
    antid = din("antiI", [64, 64])
    colmd = din("colmask", [128, 64])
    trimd = din("trimask", [128, 2, 128])
    Wd = {}
    sidx = STAGES.index(STOP) if STOP else 6
    for l in layers:
        w = {}
        w["gv"] = din("gv%d" % l, [1, NGV])
        if sidx >= 1:
            w["w_in"] = din("w_in%d" % l, [2048, 3904])
            w["rpbp"] = din("rpbp%d" % l, [60, 160])
        if sidx >= 2:
            w["uq"] = din("uq%d" % l, [768, 768])
            w["ukv"] = din("ukv%d" % l, [256, 1024])
        if sidx >= 4:
            w["mkv"] = din("mkv%d" % l, [2048, 1024])
        if sidx >= 5:
            w["wbr"] = din("wbr%d" % l, [4, 512, 2048])
            w["wg"] = din("wg%d" % l, [2048, 4, 2048])
            w["wo"] = din("wo%d" % l, [2048, 2048])
            w["bgT"] = din("bgT%d" % l, [128, 64])
        if sidx < 6:
            pass
        elif l % 2 == 0:
            w["up"] = din("ffn_up", [2048, 11264])
            w["down"] = din("ffn_down", [5632, 2048])
        else:
            w["router"] = din("router", [2048, 8])
            w["mup"] = din("moe_up", [8, 2048, 14336])
            w["mdown"] = din("moe_down", [8, 7168, 2048])
        Wd[l] = w
    hout = nc.dram_tensor("hout", [8, 128, D], F32, kind="ExternalOutput").ap()
    h1s = nc.dram_tensor("h1s", [16, 128, D], F32, kind="Internal").ap()
    hmid = nc.dram_tensor("hmid", [8, 128, D], F32, kind="Internal").ap()
    hacc = nc.dram_tensor("hacc", [8, 128, D], F32, kind="Internal").ap()
    dbg = None
    if DEBUG:
        dbg = nc.dram_tensor("dbg", [8, 128, D], F32, kind="ExternalOutput").ap()
    dram = {"xin": xin, "h1s": h1s, "hout": hout}
    Bd = {k: [[Buf()] * 4 for _ in range(16)] for k in ("xin", "h1s", "hout", "hmid", "hacc")}

    st = contextlib.ExitStack()
    with st:
        def sb(name, shape, dt):
            return st.enter_context(nc.sbuf_tensor(name, list(shape), dt))
        ARENA = 196 * 1024
        arena_t = sb("arena", [128, ARENA // 2], BF16)
        identf = sb("identf", [128, 128], F32)
        identb = sb("identb", [128, 128], BF16)
        anti = sb("anti", [64, 64], F32)
        colm = sb("colm", [128, 64], F32)
        trimf = sb("trimf", [128, 2, 128], F32)
        trim = sb("trim", [128, 2, 128], BF16)
        cst = sb("cst", [128, 16, 64], F32)
        lsel = sb("lselt", [128, 2], F32)
        ps = [st.enter_context(nc.psum_tensor("ps%d" % i, [128, 512], F32)) for i in range(8)]
        Bps = [Buf("ps%d" % i) for i in range(8)]
        S = Sched(nc)
        ar = Arena(arena_t, ARENA)
        Bconst = Buf("const")

        for dst, src in ((identf, identd), (anti, antid), (colm, colmd), (trimf, trimd), (cst, csd), (lsel, lseld)):
            S.dma("sp", I("dma_start", out=dst[:], in_=src), writes=[Buf()])
        S.barrier()
        S.op("dve", I("tensor_copy", out=identb[:], in_=identf[:]), writes=[Bconst])
        S.op("dve", I("tensor_copy", out=trim[:], in_=trimf[:]), writes=[Bconst])
        S.barrier()

        rot = {"t": 0}

        def psT(i):
            return ps[i][:, :].bitcast(BF16)

        def mm(bank, out_ap, lhsT, rhs, start, stop, reads):
            S.op("pe", I("matmul", out_ap, lhsT=lhsT, rhs=rhs, start=start, stop=stop, skip_group_check=True),
                 reads=reads, writes=[Bps[bank]])

        def wload(dst_ap, src_ap, buf):
            S.dma("pool", I("dma_start", out=dst_ap, in_=src_ap), writes=[buf])

        def bload(dst_ap, gv, sec, buf):
            o, n = GSEC[sec]
            S.dma("sp", I("dma_start", out=dst_ap, in_=gv[0:1, o:o + n].partition_broadcast(128)), writes=[buf])

        def rstd_of(ss_ap, n, dim, reads, wbuf):
            t = ar.alloc([n], F32)
            rs = ar.alloc([n], F32)
            bt = Buf()
            S.op("act", I("activation", out=t, in_=ss_ap, func=AF.Sqrt, bias=EPS, scale=1.0 / dim),
                 reads=reads, writes=[bt])
            S.op("dve", I("reciprocal", out=rs, in_=t), reads=[bt], writes=[wbuf])
            return rs

        def headnorm(zs3, Bzs, H, d, gain2, Bg, out3, Bout, sq_scr):
            n = H * d
            sq = sq_scr[:, 0:n]
            Bsq = Buf()
            S.op("dve", I("tensor_tensor", out=sq, in0=zs3.rearrange("p h d -> p (h d)"), in1=zs3.rearrange("p h d -> p (h d)"), op=ALU.mult),
                 reads=[Bzs], writes=[Bsq])
            ss = ar.alloc([H], F32)
            Bss = Buf()
            S.op("dve", I("tensor_reduce", out=ss, in_=sq.rearrange("p (h d) -> p h d", h=H), axis=AX.X, op=ALU.add),
                 reads=[Bsq], writes=[Bss])
            Brs = Buf()
            rs = rstd_of(ss, H, d, [Bss], Brs)
            S.op("dve", I("tensor_tensor", out=sq.rearrange("p (h d) -> p h d", h=H), in0=zs3, in1=bcast(rs, [128, H, d]), op=ALU.mult),
                 reads=[Bzs, Brs], writes=[Bsq])
            S.op("dve", I("tensor_tensor", out=out3.rearrange("p h d -> p (h d)"), in0=sq, in1=gain2, op=ALU.mult),
                 reads=[Bsq, Bg], writes=[Bout])
            return rs, Brs

        def rope(x3, Bx, H, ti, out3, Bout, scr):
            cos = bc_mid(cst[:, ti, 0:32], H)
            sin = bc_mid(cst[:, ti, 32:64], H)
            a = scr[:, 0:H * 32].rearrange("p (h d) -> p h d", h=H)
            b = scr[:, H * 32:H * 64].rearrange("p (h d) -> p h d", h=H)
            x1 = x3[:, :, 0:32]
            x2 = x3[:, :, 32:64]
            Bs = Buf()
            S.op("dve", I("tensor_tensor", out=a, in0=x1, in1=cos, op=ALU.mult), reads=[Bx], writes=[Bs])
            S.op("dve", I("tensor_tensor", out=b, in0=x2, in1=sin, op=ALU.mult), reads=[Bx], writes=[Bs])
            S.op("dve", I("tensor_tensor", out=out3[:, :, 0:32], in0=a, in1=b, op=ALU.subtract), reads=[Bs], writes=[Bout])
            S.op("dve", I("tensor_tensor", out=a, in0=x2, in1=cos, op=ALU.mult), reads=[Bx], writes=[Bs])
            S.op("dve", I("tensor_tensor", out=b, in0=x1, in1=sin, op=ALU.mult), reads=[Bx], writes=[Bs])
            S.op("dve", I("tensor_tensor", out=out3[:, :, 32:64], in0=a, in1=b, op=ALU.add), reads=[Bs], writes=[Bout])

        TB = (6, 7)

        def transposes(srcs, Bsrc, dst_fn, Bdst, nrows=128):
            g = 0
            while g < len(srcs):
                n = min(8, len(srcs) - g)
                bk = TB[rot["t"] % 2]
                rot["t"] += 1
                pt = psT(bk)
                for q in range(n):
                    S.op("pe", I("transpose", out=pt[0:nrows, q * 128:(q + 1) * 128], in_=srcs[g + q], identity=identb[:]),
                         reads=[Bsrc, Bconst], writes=[Bps[bk]])
                dst = dst_fn(g, n)
                S.op("act", I("copy", out=dst, in_=pt[0:nrows, 0:n * 128].rearrange("p (n t) -> p n t", n=n)),
                     reads=[Bps[bk]], writes=[Bdst])
                g += n

        def rmsnorm_tile(src_tile_ap, src_bufs, gain, Bgain, hl, Bhl, ub, Bub):
            S.dma("sp", I("dma_start", out=hl, in_=src_tile_ap), reads=src_bufs, writes=[Bhl])
            ss = ar.alloc([1], F32)
            Bss = Buf()
            S.op("act", I("activation", out=ub, in_=hl, func=AF.Square, accum_out=ss),
                 reads=[Bhl], writes=[Bub, Bss])
            Brs = Buf()
            rs = rstd_of(ss, 1, D, [Bss], Brs)
            S.op("dve", I("scalar_tensor_tensor", out=ub, in0=hl, scalar=rs[:, 0:1], in1=gain, op0=ALU.mult, op1=ALU.mult),
                 reads=[Bhl, Brs, Bgain], writes=[Bub])
            return rs, Brs

        def layer_pass(l, qoff, src_name, dst_name):
            W = Wd[l]
            src = dram[src_name]
            Bsrc = Bd[src_name]
            gv = W["gv"]
            S.barrier()
            ar.release(0)
            uT = ar.alloc([16, 2048], BF16)
            B_uT = [Buf() for _ in range(16)]
            br = ar.alloc([8, 2048], BF16)
            B_br = [Buf() for _ in range(8)]
            if STOP:
                S.op("dve", I("memset", br, 0.0), writes=B_br)
            base = ar.mark()

            gmix = ar.alloc([2048], F32)
            Bg = Buf()
            bload(gmix, gv, "mix", Bg)
            hl = [ar.alloc([2048], F32) for _ in range(2)]
            Bhl = [Buf(), Buf()]
            ub = [ar.alloc([2048], BF16) for _ in range(2)]
            Bub = [Buf(), Buf()]
            for i in range(16):
                s = i % 2
                rmsnorm_tile(src[i], Bsrc[i], gmix, Bg, hl[s], Bhl[s], ub[s], Bub[s])
                transposes([ub[s][:, k * 128:(k + 1) * 128] for k in range(16)], Bub[s],
                           lambda g0, n, i=i: uT[:, g0:g0 + n, i * 128:(i + 1) * 128], B_uT[i])
            S.barrier()
            ar.release(base)

            def stop_here(stage):
                if STOP != stage:
                    return False
                if stage == "MERGE":
                    S.dma("sp", I("dma_start", out=hout, in_=hmid), reads=[Bd["hmid"][j][0] for j in range(8)], writes=[Buf()])
                else:
                    srcv = uT[:, 0:8, :] if stage == "U" else br
                    S.dma("pool", I("dma_start", out=hout.rearrange("t p d -> p t d"), in_=srcv), writes=[Buf()])
                S.barrier()
                return True

            if stop_here("U"):
                return
            w_in = W["w_in"].rearrange("(k p) n -> p k n", p=128)

            def zproj(i, wt, Bw, ncols, bank):
                for k in range(16):
                    mm(bank, ps[bank][:, 0:ncols], uT[:, k, i * 128:(i + 1) * 128], wt[:, k, 0:ncols], k == 0, k == 15,
                       [B_uT[i], Bw])

            ZB = (0, 1)

            def attend(j, visits, nheads, dv, score_mms, exp_scale, bias_of, mask_fn, v_of, hstride, heads_per_bank,
                       PTb, BPT, out_fn):
                OB = (4, 5)
                nv = len(visits)
                for vi, v in enumerate(visits):
                    sbk = 2 + (rot["t"] % 2)
                    rot["t"] += 1
                    first = True
                    for h in range(nheads):
                        ml = score_mms(v, h)
                        for mi, (lt, rh, rd) in enumerate(ml):
                            mm(sbk, ps[sbk][:, h * 128:(h + 1) * 128], lt, rh, first, (h == nheads - 1 and mi == len(ml) - 1), rd)
                            first = False
                    pi = vi % 2
                    PT = PTb[pi]
                    bias = bias_of(v)
                    pin = ps[sbk][:, 0:nheads * 128].rearrange("p (h q) -> p h q", h=nheads)
                    if bias is None:
                        S.op("act", I("activation", out=PT, in_=pin, func=AF.Exp, scale=exp_scale),
                             reads=[Bps[sbk]], writes=[BPT[pi]])
                    else:
                        S.op("act", I("activation", out=PT, in_=pin, func=AF.Exp, bias=bias, scale=exp_scale),
                             reads=[Bps[sbk], Bconst], writes=[BPT[pi]])
                    if mask_fn is not None:
                        mask_fn(v, PT, BPT[pi])
                    for h in range(nheads):
                        ob = OB[h // heads_per_bank]
                        hh = h % heads_per_bank
                        vr, vreads = v_of(v, h)
                        mm(ob, ps[ob][:, hh * hstride:hh * hstride + dv + 1], PT[:, h, :], vr,
                           (vi == 0 and hh == 0), (vi == nv - 1), [BPT[pi]] + vreads)
                out_fn(OB)

            def finish_plain(j, nheads, dv, hstride, heads_per_bank, col0, extra_den=None):
                def fn(OB):
                    for bi in range((nheads + heads_per_bank - 1) // heads_per_bank):
                        ob = OB[bi]
                        nh = min(heads_per_bank, nheads - bi * heads_per_bank)
                        rec = ar.alloc([nh], F32)
                        Brec = Buf()
                        denv = ps[ob][:, dv:dv + (nh - 1) * hstride + 1:hstride]
                        if extra_den is None:
                            S.op("dve", I("reciprocal", out=rec, in_=denv), reads=[Bps[ob]], writes=[Brec])
                        else:
                            ex, Bex = extra_den
                            h0 = bi * heads_per_bank
                            S.op("dve", I("tensor_tensor", out=rec, in0=denv, in1=ex[:, h0:h0 + nh], op=ALU.add),
                                 reads=[Bps[ob], Bex], writes=[Brec])
                            S.op("dve", I("reciprocal", out=rec, in_=rec), reads=[Brec], writes=[Brec])
                        for hh in range(nh):
                            h = bi * heads_per_bank + hh
                            S.op("dve", I("tensor_scalar",
                                out=br[:, j, col0 + h * dv:col0 + (h + 1) * dv], in0=ps[ob][:, hh * hstride:hh * hstride + dv],
                                scalar1=rec[:, hh:hh + 1], scalar2=None, op0=ALU.mult),
                                reads=[Bps[ob], Brec], writes=[B_br[j]])
                return fn

            def zstage(n=1024):
                zs = [ar.alloc([n], F32) for _ in range(2)]
                return zs, [Buf(), Buf()]

            m0 = ar.mark()
            KT = ar.alloc([4, 2048], BF16)
            BKT = [Buf() for _ in range(16)]
            Vx = ar.alloc([16, 4, 130], BF16)
            BV = [Buf() for _ in range(16)]
            QT = ar.alloc([4, 1024], BF16)
            BQT = [Buf() for _ in range(8)]
            Mf = ar.alloc([60, 64], BF16)
            BMf = Buf()
            gq = ar.alloc([512], F32)
            gk = ar.alloc([512], F32)
            Bgq, Bgk = Buf(), Buf()
            bload(gq, gv, "naq", Bgq)
            bload(gk, gv, "nak", Bgk)
            S.op("dve", I("memset", Vx[:, :, :, 128:130], 1.0), writes=BV)
            mE = ar.mark()
            G2 = ar.alloc([15, 2, 64], F32)
            BG2 = Buf()
            et = ar.alloc([512], F32)
            Bet = Buf()
            rp = W["rpbp"]
            for hgrp in range(4):
                for a in range(2):
                    srcap = bass.AP(rp.tensor, rp.offset + hgrp * 15 * 160, [[1, 64], [160, 15], [1, 64]])
                    S.dma("sp", I("dma_start", out=G2[0:64, :, a, :], in_=srcap), writes=[BG2])
                for g0 in (0, 8):
                    n = min(8, 15 - g0)
                    bk = ZB[rot["t"] % 2]
                    rot["t"] += 1
                    for q in range(n):
                        mm(bk, ps[bk][:, q * 64:(q + 1) * 64], G2[0:64, g0 + q, :, :].rearrange("p a k -> p (a k)"), anti[:, :],
                           True, True, [BG2, Bconst])
                    S.op("act", I("activation", out=et[:, 0:n * 64], in_=ps[bk][:, 0:n * 64], func=AF.Exp),
                         reads=[Bps[bk]], writes=[Bet])
                    S.op("dve", I("tensor_tensor",
                        out=Mf[:, hgrp * 15 + g0:hgrp * 15 + g0 + n, :], in0=et[:, 0:n * 64].rearrange("p (n q) -> p n q", n=n),
                        in1=bc_mid(colm[:, :], n), op=ALU.mult), reads=[Bet, Bconst], writes=[BMf])

            S.barrier()
            ar.release(mE)
            Wc = [ar.alloc([16, 512], BF16) for _ in range(2)]
            BWc = [Buf(), Buf()]
            zs, Bzs = zstage(512)
            sq_scr = ar.alloc([512], F32)
            xb = [ar.alloc([512], BF16) for _ in range(2)]
            Bxb = [Buf(), Buf()]
            def na_cols(cb, tiles, kind):
                s = cb % 2
                wload(Wc[s], w_in[:, :, cb * 512:(cb + 1) * 512], BWc[s])
                for i in tiles:
                    bk = ZB[rot["t"] % 2]
                    rot["t"] += 1
                    zproj(i, Wc[s], BWc[s], 512, bk)
                    z = i % 2
                    if kind == "v":
                        S.op("act", I("copy", out=Vx[:, i, :, 0:128], in_=ps[bk][:, :].rearrange("p (h d) -> p h d", h=4)),
                             reads=[Bps[bk]], writes=[BV[i]])
                        continue
                    S.op("act", I("copy", out=zs[z][:, 0:512], in_=ps[bk][:, :]), reads=[Bps[bk]], writes=[Bzs[z]])
                    z3 = zs[z][:, 0:512].rearrange("p (h d) -> p h d", h=4)
                    o3 = xb[z][:, 0:512].rearrange("p (h d) -> p h d", h=4)
                    if kind == "q":
                        headnorm(z3, Bzs[z], 4, 128, gq, Bgq, o3, Bxb[z], sq_scr)
                        j = i - qoff
                        transposes([xb[z][:, h * 128:(h + 1) * 128] for h in range(4)], Bxb[z],
                                   lambda g0, n, j=j: QT[:, g0:g0 + n, j * 128:(j + 1) * 128], BQT[j])
                    else:
                        headnorm(z3, Bzs[z], 4, 128, gk, Bgk, o3, Bxb[z], sq_scr)
                        transposes([xb[z][:, h * 128:(h + 1) * 128] for h in range(4)], Bxb[z],
                                   lambda g0, n, i=i: KT[:, g0:g0 + n, i * 128:(i + 1) * 128], BKT[i])

            qtiles = list(range(qoff, qoff + 8))
            na_cols(1, range(16), "k")
            na_cols(2, range(16), "v")
            na_cols(0, qtiles, "q")
            PTb = [ar.alloc([4, 128], BF16) for _ in range(2)]
            BPT = [Buf(), Buf()]

            def rs_(r):
                return min(max(r - 4, 0), 24)

            for j in range(8):
                i = qoff + j
                visits = []
                for hc in range(2):
                    J = (i + 8 * hc) % 16
                    for c in range(16):
                        val = [[rs_(2 * J + b) <= 2 * c + a <= rs_(2 * J + b) + 7 for b in range(2)] for a in range(2)]
                        if any(val[0]) or any(val[1]):
                            visits.append((hc, J, c, (c + 8 * hc) % 16, val))

                def na_scores(v, h, j=j):
                    lc = v[3]
                    return [(KT[:, h, lc * 128:(lc + 1) * 128], QT[:, h, j * 128:(j + 1) * 128], [BKT[lc], BQT[j]])]

                def na_mask(v, PT, BP):
                    hc, J, c, lc, val = v
                    for a in range(2):
                        pa = slice(a * 64, (a + 1) * 64)
                        drp0 = 7 - 2 * c - a + 2 * J
                        if val[a][0] and val[a][1]:
                            mv = bass.AP(Mf.tensor, Mf[pa, drp0, 0:1].offset, [list(Mf.ap[0])[0:1] + [64], [15 * 64, 4], [1, 128]])
                            S.op("dve", I("tensor_tensor", out=PT[pa, :, :], in0=PT[pa, :, :], in1=mv, op=ALU.mult),
                                 reads=[BP, BMf], writes=[BP])
                        else:
                            for b in range(2):
                                qs = slice(b * 64, (b + 1) * 64)
                                if val[a][b]:
                                    mv = bass.AP(Mf.tensor, Mf[pa, drp0 + b, 0:1].offset, [list(Mf.ap[0])[0:1] + [64], [15 * 64, 4], [1, 64]])
                                    S.op("dve", I("tensor_tensor", out=PT[pa, :, qs], in0=PT[pa, :, qs], in1=mv, op=ALU.mult),
                                         reads=[BP, BMf], writes=[BP])
                                else:
                                    S.op("dve", I("memset", PT[pa, :, qs], 0.0), reads=[BP], writes=[BP])

                attend(j, visits, 4, 128, na_scores, 128 ** -0.5, lambda v: lsel[:, v[0]:v[0] + 1], na_mask,
                       lambda v, h: (Vx[:, v[3], h, 0:129], [BV[v[3]]]), 130, 2, PTb, BPT,
                       finish_plain(j, 4, 128, 130, 2, 0))
            S.barrier()
            ar.release(m0)
            if stop_here("NA"):
                return

            QTn = ar.alloc([4, 1024], BF16)
            QTr = ar.alloc([2, 1024], BF16)
            BQT = [Buf() for _ in range(8)]
            m1 = ar.mark()
            Wq = ar.alloc([16, 768], BF16)
            BWq = Buf()
            wload(Wq, w_in[:, :, 1536:2304], BWq)
            Wuq = ar.alloc([6, 768], BF16)
            BWuq = Buf()
            wload(Wuq, W["uq"].rearrange("(k p) n -> p k n", p=128), BWuq)
            gcq = ar.alloc([768], F32)
            gmq = ar.alloc([768], F32)
            Bgcq, Bgmq = Buf(), Buf()
            bload(gcq, gv, "cq", Bgcq)
            bload(gmq, gv, "mq", Bgmq)
            zs, Bzs = zstage(768)
            sq_scr = ar.alloc([768], F32)
            xb = [ar.alloc([1024], BF16) for _ in range(2)]
            Bxb = [Buf(), Buf()]
            cqT = [ar.alloc([6, 128], BF16) for _ in range(2)]
            BcqT = [Buf(), Buf()]
            qn = [ar.alloc([768], F32) for _ in range(2)]
            Bqn = [Buf(), Buf()]
            qr = [ar.alloc([256], F32) for _ in range(2)]
            Bqr = [Buf(), Buf()]
            for j in range(8):
                i = qoff + j
                z = j % 2
                b0, b1 = ZB
                for k in range(16):
                    mm(b0, ps[b0][:, :], uT[:, k, i * 128:(i + 1) * 128], Wq[:, k, 0:512], k == 0, k == 15, [B_uT[i], BWq])
                for k in range(16):
                    mm(b1, ps[b1][:, 0:256], uT[:, k, i * 128:(i + 1) * 128], Wq[:, k, 512:768], k == 0, k == 15, [B_uT[i], BWq])
                S.op("act", I("copy", out=zs[z][:, 0:512], in_=ps[b0][:, :]), reads=[Bps[b0]], writes=[Bzs[z]])
                S.op("act", I("copy", out=zs[z][:, 512:768], in_=ps[b1][:, 0:256]), reads=[Bps[b1]], writes=[Bzs[z]])
                headnorm(zs[z][:, 0:768].rearrange("p (h d) -> p h d", h=1), Bzs[z], 1, 768, gcq, Bgcq,
                         xb[z][:, 0:768].rearrange("p (h d) -> p h d", h=1), Bxb[z], sq_scr)
                transposes([xb[z][:, c * 128:(c + 1) * 128] for c in range(6)], Bxb[z],
                           lambda g0, n, z=z: cqT[z][:, g0:g0 + n, :], BcqT[z])
                for c in range(6):
                    mm(b0, ps[b0][:, :], cqT[z][:, c, :], Wuq[:, c, 0:512], c == 0, c == 5, [BcqT[z], BWuq])
                for c in range(6):
                    mm(b1, ps[b1][:, 0:256], cqT[z][:, c, :], Wuq[:, c, 512:768], c == 0, c == 5, [BcqT[z], BWuq])
                S.op("act", I("copy", out=zs[z][:, 0:512], in_=ps[b0][:, :]), reads=[Bps[b0]], writes=[Bzs[z]])
                S.op("act", I("copy", out=zs[z][:, 512:768], in_=ps[b1][:, 0:256]), reads=[Bps[b1]], writes=[Bzs[z]])
                q3 = qn[z][:, :].rearrange("p (h d) -> p h d", h=4)
                headnorm(zs[z][:, 0:768].rearrange("p (h d) -> p h d", h=4), Bzs[z], 4, 192, gmq, Bgmq, q3, Bqn[z], sq_scr)
                S.op("dve", I("tensor_copy", out=xb[z][:, 0:512].rearrange("p (h d) -> p h d", h=4), in_=q3[:, :, 0:128]),
                     reads=[Bqn[z]], writes=[Bxb[z]])
                S.op("dve", I("tensor_copy", out=qr[z][:, :].rearrange("p (h d) -> p h d", h=4), in_=q3[:, :, 128:192]),
                     reads=[Bqn[z]], writes=[Bqr[z]])
                rope(qr[z][:, :].rearrange("p (h d) -> p h d", h=4), Bqr[z], 4, i,
                     xb[z][:, 512:768].rearrange("p (h d) -> p h d", h=4), Bxb[z], sq_scr)
                transposes([xb[z][:, h * 128:(h + 1) * 128] for h in range(4)], Bxb[z],
                           lambda g0, n, j=j: QTn[:, g0:g0 + n, j * 128:(j + 1) * 128], BQT[j])
                transposes([xb[z][:, 512 + p * 128:512 + (p + 1) * 128] for p in range(2)], Bxb[z],
                           lambda g0, n, j=j: QTr[:, g0:g0 + n, j * 128:(j + 1) * 128], BQT[j])
            S.barrier()
            ar.release(m1)
            KTn = ar.alloc([4, 2048], BF16)
            KTr = ar.alloc([2, 2048], BF16)
            BKT = [Buf() for _ in range(16)]
            Vx = ar.alloc([16, 4, 130], BF16)
            BV = [Buf() for _ in range(16)]
            S.op("dve", I("memset", Vx[:, :, :, 128:130], 1.0), writes=BV)
            m1 = ar.mark()
            Wk = ar.alloc([16, 320], BF16)
            BWk = Buf()
            wload(Wk, w_in[:, :, 2304:2624], BWk)
            Wukv = ar.alloc([2, 1024], BF16)
            BWukv = Buf()
            wload(Wukv, W["ukv"].rearrange("(k p) n -> p k n", p=128), BWukv)
            gckv = ar.alloc([256], F32)
            gkn = ar.alloc([512], F32)
            gkr = ar.alloc([64], F32)
            Bgc, Bgn, Bgr = Buf(), Buf(), Buf()
            bload(gckv, gv, "ckv", Bgc)
            bload(gkn, gv, "mkn", Bgn)
            bload(gkr, gv, "mkr", Bgr)
            zs, Bzs = zstage(320)
            sq_scr = ar.alloc([768], F32)
            xb = [ar.alloc([1024], BF16) for _ in range(2)]
            Bxb = [Buf(), Buf()]
            ckT = [ar.alloc([2, 128], BF16) for _ in range(2)]
            BckT = [Buf(), Buf()]
            kvs = [ar.alloc([1024], F32) for _ in range(2)]
            Bkvs = [Buf(), Buf()]
            kr = [ar.alloc([256], F32) for _ in range(2)]
            Bkr = [Buf(), Buf()]
            for i in range(16):
                z = i % 2
                bk = ZB[rot["t"] % 2]
                rot["t"] += 1
                zproj(i, Wk, BWk, 320, bk)
                S.op("act", I("copy", out=zs[z][:, 0:320], in_=ps[bk][:, 0:320]), reads=[Bps[bk]], writes=[Bzs[z]])
                headnorm(zs[z][:, 0:256].rearrange("p (h d) -> p h d", h=1), Bzs[z], 1, 256, gckv, Bgc,
                         xb[z][:, 0:256].rearrange("p (h d) -> p h d", h=1), Bxb[z], sq_scr)
                transposes([xb[z][:, c * 128:(c + 1) * 128] for c in range(2)], Bxb[z],
                           lambda g0, n, z=z: ckT[z][:, g0:g0 + n, :], BckT[z])
                b0 = ZB[rot["t"] % 2]
                rot["t"] += 1
                b1 = ZB[rot["t"] % 2]
                rot["t"] += 1
                for half, bb in ((0, b0), (1, b1)):
                    for c in range(2):
                        mm(bb, ps[bb][:, :], ckT[z][:, c, :], Wukv[:, c, half * 512:(half + 1) * 512], c == 0, c == 1, [BckT[z], BWukv])
                    S.op("act", I("copy", out=kvs[z][:, half * 512:(half + 1) * 512], in_=ps[bb][:, :]),
                         reads=[Bps[bb]], writes=[Bkvs[z]])
                kv4 = kvs[z][:, :].rearrange("p (h t d) -> p h t d", h=4, t=2)
                S.op("dve", I("tensor_copy", out=Vx[:, i, :, 0:128], in_=kv4[:, :, 1, :]), reads=[Bkvs[z]], writes=[BV[i]])
                sqv = sq_scr[:, 0:512].rearrange("p (h d) -> p h d", h=4)
                Bsq = Buf()
                S.op("dve", I("tensor_tensor", out=sqv, in0=kv4[:, :, 0, :], in1=kv4[:, :, 0, :], op=ALU.mult),
                     reads=[Bkvs[z]], writes=[Bsq])
                ss = ar.alloc([8], F32)
                Bss = Buf()
                S.op("dve", I("tensor_reduce", out=ss[:, 0:4], in_=sqv, axis=AX.X, op=ALU.add), reads=[Bsq], writes=[Bss])
                S.op("act", I("activation", out=sq_scr[:, 512:576], in_=zs[z][:, 256:320], func=AF.Square, accum_out=ss[:, 4:5]),
                     reads=[Bzs[z]], writes=[Bss, Bsq])
                S.op("dve", I("tensor_scalar", out=ss[:, 0:4], in0=ss[:, 0:4], scalar1=ss[:, 4:5], scalar2=None, op0=ALU.add),
                     reads=[Bss], writes=[Bss])
                Brs = Buf()
                rs = rstd_of(ss[:, 0:4], 4, 192, [Bss], Brs)
                S.op("dve", I("tensor_tensor", out=sqv, in0=kv4[:, :, 0, :], in1=bcast(rs, [128, 4, 128]), op=ALU.mult),
                     reads=[Bkvs[z], Brs], writes=[Bsq])
                S.op("dve", I("tensor_tensor", out=xb[z][:, 0:512], in0=sq_scr[:, 0:512], in1=gkn, op=ALU.mult),
                     reads=[Bsq, Bgn], writes=[Bxb[z]])
                S.op("dve", I("tensor_tensor", out=sq_scr[:, 576:640], in0=zs[z][:, 256:320], in1=gkr, op=ALU.mult),
                     reads=[Bzs[z], Bgr], writes=[Bsq])
                rope(sq_scr[:, 576:640].rearrange("p (h d) -> p h d", h=1), Bsq, 1, i,
                     kr[z][:, 0:64].rearrange("p (h d) -> p h d", h=1), Bkr[z], sq_scr[:, 640:768])
                S.op("dve", I("tensor_tensor", out=xb[z][:, 512:768].rearrange("p (h d) -> p h d", h=4),
                                                                  in0=bc_mid(kr[z][:, 0:64], 4), in1=bcast(rs, [128, 4, 64]), op=ALU.mult),
                     reads=[Bkr[z], Brs], writes=[Bxb[z]])
                transposes([xb[z][:, h * 128:(h + 1) * 128] for h in range(4)], Bxb[z],
                           lambda g0, n, i=i: KTn[:, g0:g0 + n, i * 128:(i + 1) * 128], BKT[i])
                transposes([xb[z][:, 512 + p * 128:512 + (p + 1) * 128] for p in range(2)], Bxb[z],
                           lambda g0, n, i=i: KTr[:, g0:g0 + n, i * 128:(i + 1) * 128], BKT[i])
            S.barrier()
            ar.release(m1)
            PTb = [ar.alloc([4, 128], BF16) for _ in range(2)]
            BPT = [Buf(), Buf()]
            for j in range(8):
                def mla_scores(v, h, j=j):
                    lc = v
                    pr = slice((h % 2) * 64, (h % 2) * 64 + 64)
                    return [(KTn[:, h, lc * 128:(lc + 1) * 128], QTn[:, h, j * 128:(j + 1) * 128], [BKT[lc], BQT[j]]),
                            (KTr[pr, h // 2, lc * 128:(lc + 1) * 128], QTr[pr, h // 2, j * 128:(j + 1) * 128], [BKT[lc], BQT[j]])]
                attend(j, list(range(16)), 4, 128, mla_scores, 192 ** -0.5, lambda v: None, None,
                       lambda v, h: (Vx[:, v, h, 0:129], [BV[v]]), 130, 2, PTb, BPT,
                       finish_plain(j, 4, 128, 130, 2, 512))
            S.barrier()
            ar.release(m0)
            if stop_here("MLA"):
                return

            KTs = ar.alloc([4, 2048], BF16)
            BKT = [Buf() for _ in range(16)]
            Vs = ar.alloc([16, 2, 66], BF16)
            BV = [Buf() for _ in range(16)]
            QTs = ar.alloc([4, 1024], BF16)
            BQT = [Buf() for _ in range(8)]
            S.op("dve", I("memset", Vs[:, :, :, 64:66], 1.0), writes=BV)
            gsq = ar.alloc([512], F32)
            gsk = ar.alloc([128], F32)
            esink = ar.alloc([64], F32)
            Bgsq, Bgsk, Besk = Buf(), Buf(), Buf()
            bload(gsq, gv, "sq", Bgsq)
            bload(gsk, gv, "sk", Bgsk)
            bload(esink, gv, "sink", Besk)
            S.op("act", I("activation", out=esink, in_=esink, func=AF.Exp), reads=[Besk], writes=[Besk])
            Wk = ar.alloc([16, 256], BF16)
            BWk = Buf()
            wload(Wk, w_in[:, :, 3136:3392], BWk)
            Wq = ar.alloc([16, 512], BF16)
            BWq = Buf()
            wload(Wq, w_in[:, :, 2624:3136], BWq)
            zs, Bzs = zstage()
            sq_scr = ar.alloc([1024], F32)
            xb = [ar.alloc([1024], BF16) for _ in range(2)]
            Bxb = [Buf(), Buf()]
            xn = [ar.alloc([512], F32) for _ in range(2)]
            Bxn = [Buf(), Buf()]
            for i in range(16):
                z = i % 2
                bk = ZB[rot["t"] % 2]
                rot["t"] += 1
                zproj(i, Wk, BWk, 256, bk)
                S.op("act", I("copy", out=zs[z][:, 0:256], in_=ps[bk][:, 0:256]), reads=[Bps[bk]], writes=[Bzs[z]])
                S.op("dve", I("tensor_copy", out=Vs[:, i, :, 0:64], in_=zs[z][:, 128:256].rearrange("p (h d) -> p h d", h=2)),
                     reads=[Bzs[z]], writes=[BV[i]])
                k3 = xn[z][:, 0:128].rearrange("p (h d) -> p h d", h=2)
                headnorm(zs[z][:, 0:128].rearrange("p (h d) -> p h d", h=2), Bzs[z], 2, 64, gsk, Bgsk, k3, Bxn[z], sq_scr)
                if i < 2:
                    S.op("dve", I("memset", xb[z][:, 0:512], 0.0), writes=[Bxb[z]])
                x5 = xb[z][:, 0:512].rearrange("p (h v d) -> p h v d", h=2, v=2)
                rope(k3, Bxn[z], 2, i, x5[:, :, 0, 0:64], Bxb[z], sq_scr)
                S.op("dve", I("tensor_copy", out=x5[:, :, 1, 64:128], in_=x5[:, :, 0, 0:64]), reads=[Bxb[z]], writes=[Bxb[z]])
                transposes([xb[z][:, h * 128:(h + 1) * 128] for h in range(4)], Bxb[z],
                           lambda g0, n, i=i: KTs[:, g0:g0 + n, i * 128:(i + 1) * 128], BKT[i])
            for j in range(8):
                i = qoff + j
                z = j % 2
                bk = ZB[rot["t"] % 2]
                rot["t"] += 1
                zproj(i, Wq, BWq, 512, bk)
                S.op("act", I("copy", out=zs[z][:, 0:512], in_=ps[bk][:, :]), reads=[Bps[bk]], writes=[Bzs[z]])
                q3 = xn[z][:, 0:512].rearrange("p (h d) -> p h d", h=8)
                headnorm(zs[z][:, 0:512].rearrange("p (h d) -> p h d", h=8), Bzs[z], 8, 64, gsq, Bgsq, q3, Bxn[z], sq_scr)
                rope(q3, Bxn[z], 8, i, xb[z][:, 0:512].rearrange("p (h d) -> p h d", h=8), Bxb[z], sq_scr)
                transposes([xb[z][:, p * 128:(p + 1) * 128] for p in range(4)], Bxb[z],
                           lambda g0, n, j=j: QTs[:, g0:g0 + n, j * 128:(j + 1) * 128], BQT[j])
            PTb = [ar.alloc([4, 128], BF16) for _ in range(2)]
            BPT = [Buf(), Buf()]
            for j in range(8):
                i = qoff + j
                for hk in range(2):
                    visits = []
                    for hc in range(2):
                        J = (i + 8 * hc) % 16
                        for rel in (-1, 0, 1):
                            c = J + rel
                            if 0 <= c <= 15:
                                visits.append((hc, rel, (c + 8 * hc) % 16))

                    def sw_scores(v, g, j=j, hk=hk):
                        lc = v[2]
                        hq = hk * 4 + g
                        pr = slice((hq % 2) * 64, (hq % 2) * 64 + 64)
                        return [(KTs[:, hk * 2 + (hq % 2), lc * 128:(lc + 1) * 128], QTs[:, hq // 2, j * 128:(j + 1) * 128], [BKT[lc], BQT[j]])]

                    def sw_mask(v, PT, BP):
                        if v[1] == 0:
                            return
                        mi = 0 if v[1] == -1 else 1
                        S.op("dve", I("tensor_tensor", out=PT, in0=PT, in1=bc_mid(trim[:, mi, :], 4), op=ALU.mult),
                             reads=[BP, Bconst], writes=[BP])

                    def sw_finish(OB, j=j, hk=hk):
                        ob = OB[0]
                        rec = ar.alloc([4], F32)
                        Brec = Buf()
                        denv = ps[ob][:, 64:64 + 3 * 66 + 1:66]
                        S.op("dve", I("tensor_tensor", out=rec, in0=denv, in1=esink[:, hk * 4:hk * 4 + 4], op=ALU.add),
                             reads=[Bps[ob], Besk], writes=[Brec])
                        S.op("dve", I("reciprocal", out=rec, in_=rec), reads=[Brec], writes=[Brec])
                        for g in range(4):
                            hq = hk * 4 + g
                            S.op("dve", I("tensor_scalar",
                                out=br[:, j, 1024 + hq * 64:1024 + (hq + 1) * 64], in0=ps[ob][:, g * 66:g * 66 + 64],
                                scalar1=rec[:, g:g + 1], scalar2=None, op0=ALU.mult), reads=[Bps[ob], Brec], writes=[B_br[j]])

                    attend(j, visits, 4, 64, sw_scores, 64 ** -0.5, lambda v: lsel[:, v[0]:v[0] + 1], sw_mask,
                           lambda v, g, hk=hk: (Vs[:, v[2], hk, 0:65], [BV[v[2]]]), 66, 4, PTb, BPT, sw_finish)
            S.barrier()
            ar.release(m0)
            if stop_here("SWA"):
                return

            KTm = ar.alloc([4, 256], BF16)
            BKT = [Buf(), Buf()]
            Vm = ar.alloc([2, 4, 130], BF16)
            BV = [Buf(), Buf()]
            QTm = ar.alloc([4, 1024], BF16)
            BQT = [Buf() for _ in range(8)]
            S.op("dve", I("memset", Vm[:, :, :, 128:130], 1.0), writes=BV)
            memT = ar.alloc([16, 256], BF16)
            BmT = [Buf(), Buf()]
            gmq2 = ar.alloc([512], F32)
            gmk = ar.alloc([512], F32)
            Bgmn, Bgmq2, Bgmk = Buf(), Buf(), Buf()
            bload(gmq2, gv, "memq", Bgmq2)
            bload(gmk, gv, "memk", Bgmk)
            zs, Bzs = zstage(512)
            sq_scr = ar.alloc([512], F32)
            xb = [ar.alloc([512], BF16) for _ in range(2)]
            Bxb = [Buf(), Buf()]
            mM = ar.mark()
            gmn = ar.alloc([2048], F32)
            bload(gmn, gv, "memn", Bgmn)
            hl = [ar.alloc([2048], F32) for _ in range(2)]
            Bhl = [Buf(), Buf()]
            ub = [ar.alloc([2048], BF16) for _ in range(2)]
            Bub = [Buf(), Buf()]
            Wc = [ar.alloc([16, 512], BF16) for _ in range(2)]
            BWc = [Buf(), Buf()]
            mkv = W["mkv"].rearrange("(k p) n -> p k n", p=128)
            wload(Wc[0], mkv[:, :, 0:512], BWc[0])
            wload(Wc[1], mkv[:, :, 512:1024], BWc[1])
            for t in range(2):
                rmsnorm_tile(memd[t], [], gmn, Bgmn, hl[t], Bhl[t], ub[t], Bub[t])
                transposes([ub[t][:, k * 128:(k + 1) * 128] for k in range(16)], Bub[t],
                           lambda g0, n, t=t: memT[:, g0:g0 + n, t * 128:(t + 1) * 128], BmT[t])
                for half in range(2):
                    bk = ZB[rot["t"] % 2]
                    rot["t"] += 1
                    for k in range(16):
                        mm(bk, ps[bk][:, :], memT[:, k, t * 128:(t + 1) * 128], Wc[half][:, k, :], k == 0, k == 15, [BmT[t], BWc[half]])
                    if half == 1:
                        S.op("act", I("copy", out=Vm[:, t, :, 0:128], in_=ps[bk][:, :].rearrange("p (h d) -> p h d", h=4)),
                             reads=[Bps[bk]], writes=[BV[t]])
                    else:
                        S.op("act", I("copy", out=zs[t][:, 0:512], in_=ps[bk][:, :]), reads=[Bps[bk]], writes=[Bzs[t]])
                        headnorm(zs[t][:, 0:512].rearrange("p (h d) -> p h d", h=4), Bzs[t], 4, 128, gmk, Bgmk,
                                 xb[t][:, 0:512].rearrange("p (h d) -> p h d", h=4), Bxb[t], sq_scr)
                        transposes([xb[t][:, h * 128:(h + 1) * 128] for h in range(4)], Bxb[t],
                                   lambda g0, n, t=t: KTm[:, g0:g0 + n, t * 128:(t + 1) * 128], BKT[t])
            S.barrier()
            ar.release(mM)
            Wq = ar.alloc([16, 512], BF16)
            BWq = Buf()
            wload(Wq, w_in[:, :, 3392:3904], BWq)
            for j in range(8):
                i = qoff + j
                z = j % 2
                bk = ZB[rot["t"] % 2]
                rot["t"] += 1
                zproj(i, Wq, BWq, 512, bk)
                S.op("act", I("copy", out=zs[z][:, 0:512], in_=ps[bk][:, :]), reads=[Bps[bk]], writes=[Bzs[z]])
                headnorm(zs[z][:, 0:512].rearrange("p (h d) -> p h d", h=4), Bzs[z], 4, 128, gmq2, Bgmq2,
                         xb[z][:, 0:512].rearrange("p (h d) -> p h d", h=4), Bxb[z], sq_scr)
                transposes([xb[z][:, h * 128:(h + 1) * 128] for h in range(4)], Bxb[z],
                           lambda g0, n, j=j: QTm[:, g0:g0 + n, j * 128:(j + 1) * 128], BQT[j])
            PTb = [ar.alloc([4, 128], BF16) for _ in range(2)]
            BPT = [Buf(), Buf()]
            for j in range(8):
                def me_scores(v, h, j=j):
                    return [(KTm[:, h, v * 128:(v + 1) * 128], QTm[:, h, j * 128:(j + 1) * 128], [BKT[v], BQT[j]])]
                attend(j, [0, 1], 4, 128, me_scores, 128 ** -0.5, lambda v: None, None,
                       lambda v, h: (Vm[:, v, h, 0:129], [BV[v]]), 130, 2, PTb, BPT,
                       finish_plain(j, 4, 128, 130, 2, 1536))
            S.barrier()
            ar.release(base)

            if stop_here("MEM"):
                return

            brT = ar.alloc([16, 1024], BF16)
            BbrT = [Buf() for _ in range(8)]
            for j in range(8):
                transposes([br[:, j, c * 128:(c + 1) * 128] for c in range(16)], B_br[j],
                           lambda g0, n, j=j: brT[:, g0:g0 + n, j * 128:(j + 1) * 128], BbrT[j])
            S.barrier()
            mT = br.rearrange("p a b -> p (a b)").rearrange("p (a b) -> p a b", a=16)
            BmTt = [Buf() for _ in range(8)]
            acc = ar.alloc([4, 2, 512], F32)
            Bacc = [[Buf(), Buf()] for _ in range(4)]
            Wg = [ar.alloc([16, 512], BF16) for _ in range(2)]
            BWg = [Buf(), Buf()]
            Wb = [ar.alloc([4, 512], BF16) for _ in range(2)]
            BWb = [Buf(), Buf()]
            sgt = [ar.alloc([512], F32) for _ in range(2)]
            Bsg = [Buf(), Buf()]
            bgt = ar.alloc([64], F32)
            Bbg = Buf()
            S.dma("sp", I("dma_start", out=bgt, in_=W["bgT"]), writes=[Bbg])
            rb = [ar.alloc([512], F32) for _ in range(2)]
            Brb = [Buf(), Buf()]
            obf = sgt
            Bob = Bsg
            wgv = W["wg"].rearrange("(k p) n d -> p k n d", p=128)
            wbv = W["wbr"].rearrange("n (c p) d -> p n c d", p=128)
            cnt = 0
            for dblk in range(4):
                for n in range(4):
                    s = cnt % 2
                    cnt += 1
                    wload(Wg[s], wgv[:, :, n, dblk * 512:(dblk + 1) * 512], BWg[s])
                    wload(Wb[s], wbv[:, n, :, dblk * 512:(dblk + 1) * 512], BWb[s])
                    for dcl in range(4):
                        dc = dblk * 4 + dcl
                        for tb in range(2):
                            bp = rot["t"] % 2
                            bg = 2 + rot["t"] % 2
                            rot["t"] += 1
                            for c in range(4):
                                mm(bp, ps[bp][:, :], Wb[s][:, c, dcl * 128:(dcl + 1) * 128], brT[:, n * 4 + c, tb * 512:(tb + 1) * 512],
                                   c == 0, c == 3, [BWb[s]] + BbrT[tb * 4:tb * 4 + 4])
                            t0 = qoff * 128 + tb * 512
                            for k in range(16):
                                mm(bg, ps[bg][:, :], Wg[s][:, k, dcl * 128:(dcl + 1) * 128], uT[:, k, t0:t0 + 512],
                                   k == 0, k == 15, [BWg[s]] + B_uT[qoff + tb * 4:qoff + tb * 4 + 4])
                            sg = sgt[tb]
                            S.op("act", I("activation", out=sg, in_=ps[bg][:, :], func=AF.Sigmoid,
                                                                                         bias=bgt[:, n * 16 + dc:n * 16 + dc + 1], scale=1.0),
                                 reads=[Bps[bg], Bbg], writes=[Bsg[tb]])
                            if n == 0:
                                S.op("dve", I("tensor_tensor", out=acc[:, dcl, tb, :], in0=sg, in1=ps[bp][:, :], op=ALU.mult),
                                     reads=[Bsg[tb], Bps[bp]], writes=[Bacc[dcl][tb]])
                            else:
                                S.op("dve", I("tensor_tensor", out=sg, in0=sg, in1=ps[bp][:, :], op=ALU.mult),
                                     reads=[Bsg[tb], Bps[bp]], writes=[Bsg[tb]])
                                if n < 3:
                                    S.op("dve", I("tensor_tensor", out=acc[:, dcl, tb, :], in0=acc[:, dcl, tb, :], in1=sg, op=ALU.add),
                                         reads=[Bsg[tb], Bacc[dcl][tb]], writes=[Bacc[dcl][tb]])
                                else:
                                    S.op("dve", I("tensor_tensor", out=mT[:, dc, tb * 512:(tb + 1) * 512], in0=acc[:, dcl, tb, :], in1=sg, op=ALU.add),
                                         reads=[Bsg[tb], Bacc[dcl][tb]], writes=BmTt[tb * 4:tb * 4 + 4])
            wov = W["wo"].rearrange("(k p) n -> p k n", p=128)
            for cb in range(4):
                s = cnt % 2
                cnt += 1
                wload(Wg[s], wov[:, :, cb * 512:(cb + 1) * 512], BWg[s])
                for j in range(8):
                    bk = rot["t"] % 4
                    rot["t"] += 1
                    for dc in range(16):
                        mm(bk, ps[bk][:, :], mT[:, dc, j * 128:(j + 1) * 128], Wg[s][:, dc, :], dc == 0, dc == 15, [BmTt[j], BWg[s]])
                    r = (j + cb) % 2
                    S.dma("sp", I("dma_start", out=rb[r], in_=src[qoff + j][:, cb * 512:(cb + 1) * 512]),
                          reads=[Bsrc[qoff + j][cb]], writes=[Brb[r]])
                    S.op("dve", I("tensor_tensor", out=obf[r], in0=ps[bk][:, :], in1=rb[r], op=ALU.add),
                         reads=[Bps[bk], Brb[r]], writes=[Bob[r]])
                    S.dma("sp", I("dma_start", out=hmid[j][:, cb * 512:(cb + 1) * 512], in_=obf[r]),
                          reads=[Bob[r]], writes=[Bd["hmid"][j][cb]])
            S.barrier()
            ar.release(0)
            if stop_here("MERGE"):
                return

            dst = dram[dst_name]
            dtile0 = qoff if dst_name == "h1s" else 0
            Bdst = Bd[dst_name]
            hnT = ar.alloc([16, 1024], BF16)
            BhnT = [Buf() for _ in range(8)]
            comb = ar.alloc([8, 8], F32)
            Bcomb = [Buf() for _ in range(8)]
            m2 = ar.mark()
            gff = ar.alloc([2048], F32)
            Bgff = Buf()
            bload(gff, gv, "ffn", Bgff)
            hl = [ar.alloc([2048], F32) for _ in range(2)]
            Bhl = [Buf(), Buf()]
            ub = [ar.alloc([2048], BF16) for _ in range(2)]
            Bub = [Buf(), Buf()]
            moe = (l % 2 == 1)
            if moe:
                wr = ar.alloc([16, 8], F32)
                Bwr = Buf()
                S.dma("sp", I("dma_start", out=wr, in_=W["router"].rearrange("(k p) n -> p k n", p=128)), writes=[Bwr])
                hn32 = [ar.alloc([2048], F32) for _ in range(2)]
                Bhn32 = [Buf(), Buf()]
                hT32 = [ar.alloc([16, 128], F32) for _ in range(2)]
                BhT32 = [Buf(), Buf()]
            for j in range(8):
                s = j % 2
                rs, Brs = rmsnorm_tile(hmid[j], Bd["hmid"][j], gff, Bgff, hl[s], Bhl[s], ub[s], Bub[s])
                transposes([ub[s][:, k * 128:(k + 1) * 128] for k in range(16)], Bub[s],
                           lambda g0, n, j=j: hnT[:, g0:g0 + n, j * 128:(j + 1) * 128], BhnT[j])
                if moe:
                    S.op("dve", I("scalar_tensor_tensor", out=hn32[s], in0=hl[s], scalar=rs[:, 0:1], in1=gff, op0=ALU.mult, op1=ALU.mult),
                         reads=[Bhl[s], Brs, Bgff], writes=[Bhn32[s]])
                    for g in range(4):
                        bk = rot["t"] % 2
                        rot["t"] += 1
                        for q in range(4):
                            k = g * 4 + q
                            S.op("pe", I("transpose", out=ps[bk][:, q * 128:(q + 1) * 128], in_=hn32[s][:, k * 128:(k + 1) * 128], identity=identf[:]),
                                 reads=[Bhn32[s], Bconst], writes=[Bps[bk]])
                        S.op("act", I("copy", out=hT32[s][:, g * 4:(g + 1) * 4, :], in_=ps[bk][:, :].rearrange("p (n t) -> p n t", n=4)),
                             reads=[Bps[bk]], writes=[BhT32[s]])
                    bk = 2 + rot["t"] % 2
                    rot["t"] += 1
                    for k in range(16):
                        mm(bk, ps[bk][:, 0:8], hT32[s][:, k, :], wr[:, k, :], k == 0, k == 15, [BhT32[s], Bwr])
                    lg = ar.alloc([8], F32)
                    e1 = ar.alloc([8], F32)
                    l2 = ar.alloc([8], F32)
                    e2 = ar.alloc([8], F32)
                    mx = ar.alloc([4], F32)
                    Bt = Buf()
                    S.op("dve", I("tensor_copy", out=lg, in_=ps[bk][:, 0:8]), reads=[Bps[bk]], writes=[Bt])
                    S.op("dve", I("tensor_reduce", out=mx[:, 0:1], in_=lg, axis=AX.X, op=ALU.max), reads=[Bt], writes=[Bt])
                    S.op("dve", I("tensor_scalar", out=e1, in0=lg, scalar1=mx[:, 0:1], scalar2=None, op0=ALU.is_equal), reads=[Bt], writes=[Bt])
                    S.op("dve", I("scalar_tensor_tensor", out=l2, in0=e1, scalar=-1e30, in1=lg, op0=ALU.mult, op1=ALU.add), reads=[Bt], writes=[Bt])
                    S.op("dve", I("tensor_reduce", out=mx[:, 1:2], in_=l2, axis=AX.X, op=ALU.max), reads=[Bt], writes=[Bt])
                    S.op("dve", I("tensor_scalar", out=e2, in0=l2, scalar1=mx[:, 1:2], scalar2=None, op0=ALU.is_equal), reads=[Bt], writes=[Bt])
                    S.op("dve", I("tensor_tensor", out=mx[:, 2:3], in0=mx[:, 0:1], in1=mx[:, 1:2], op=ALU.subtract), reads=[Bt], writes=[Bt])
                    S.op("act", I("activation", out=mx[:, 3:4], in_=mx[:, 2:3], func=AF.Sigmoid, scale=-1.0), reads=[Bt], writes=[Bt])
                    S.op("act", I("activation", out=mx[:, 2:3], in_=mx[:, 2:3], func=AF.Sigmoid), reads=[Bt], writes=[Bt])
                    S.op("dve", I("tensor_scalar", out=e1, in0=e1, scalar1=mx[:, 2:3], scalar2=None, op0=ALU.mult), reads=[Bt], writes=[Bt])
                    S.op("dve", I("scalar_tensor_tensor", out=comb[:, j, :], in0=e2, scalar=mx[:, 3:4], in1=e1, op0=ALU.mult, op1=ALU.add),
                         reads=[Bt], writes=[Bcomb[j]])
            S.barrier()
            ar.release(m2)

            units = []
            if not moe:
                upv = W["up"].rearrange("(k p) n -> p k n", p=128)
                dnv = W["down"].rearrange("(f p) n -> p f n", p=128)
                for hh in range(2):
                    units.append((upv[:, :, hh * 2816:(hh + 1) * 2816], upv[:, :, 5632 + hh * 2816:5632 + (hh + 1) * 2816],
                                  dnv[:, hh * 22:(hh + 1) * 22, :], 22, None))
            else:
                for ex in range(8):
                    upv = W["mup"][ex].rearrange("(k p) n -> p k n", p=128)
                    dnv = W["mdown"][ex].rearrange("(f p) n -> p f n", p=128)
                    for hh in range(2):
                        units.append((upv[:, :, hh * 3584:(hh + 1) * 3584], upv[:, :, 7168 + hh * 3584:7168 + (hh + 1) * 3584],
                                      dnv[:, hh * 28:(hh + 1) * 28, :], 28, ex))
            FcM = units[0][3]
            actT = ar.alloc([FcM, 1024], BF16)
            Bact = [Buf() for _ in range(FcM)]
            Wu = [ar.alloc([2, 16, 256], BF16) for _ in range(2)]
            BWu = [[Buf(), Buf()], [Buf(), Buf()]]
            Wdn = [ar.alloc([FcM, 512], BF16) for _ in range(2)]
            BWdn = [Buf(), Buf()]
            sgt = [ar.alloc([512], F32) for _ in range(2)]
            Bsg = [Buf(), Buf()]
            rb = [ar.alloc([512], F32) for _ in range(2)]
            Brb = [Buf(), Buf()]
            obf = [ar.alloc([512], F32) for _ in range(2)]
            Bob = [Buf(), Buf()]
            cu = 0
            cd = 0
            for ui, (ug, uu, dn, Fc, ex) in enumerate(units):
                if ui > 0:
                    S.barrier()
                for fg in range(Fc // 2):
                    s = cu % 2
                    cu += 1
                    S.dma("pool", I("dma_start", out=Wu[s][:, 0, :, :], in_=ug[:, :, fg * 256:(fg + 1) * 256]), writes=[BWu[s][0]])
                    S.dma("pool", I("dma_start", out=Wu[s][:, 1, :, :], in_=uu[:, :, fg * 256:(fg + 1) * 256]), writes=[BWu[s][1]])
                    for fl in range(2):
                        f = fg * 2 + fl
                        for tb in range(2):
                            bgk = rot["t"] % 2
                            buk = 2 + rot["t"] % 2
                            rot["t"] += 1
                            for k in range(16):
                                mm(bgk, ps[bgk][:, :], Wu[s][:, 0, k, fl * 128:(fl + 1) * 128], hnT[:, k, tb * 512:(tb + 1) * 512],
                                   k == 0, k == 15, [BWu[s][0]] + BhnT[tb * 4:tb * 4 + 4])
                            for k in range(16):
                                mm(buk, ps[buk][:, :], Wu[s][:, 1, k, fl * 128:(fl + 1) * 128], hnT[:, k, tb * 512:(tb + 1) * 512],
                                   k == 0, k == 15, [BWu[s][1]] + BhnT[tb * 4:tb * 4 + 4])
                            sg = sgt[tb]
                            S.op("act", I("activation", out=sg, in_=ps[bgk][:, :], func=AF.Silu), reads=[Bps[bgk]], writes=[Bsg[tb]])
                            S.op("dve", I("tensor_tensor", out=actT[:, f, tb * 512:(tb + 1) * 512], in0=sg, in1=ps[buk][:, :], op=ALU.mult),
                                 reads=[Bsg[tb], Bps[buk]], writes=[Bact[f]])
                last = (ui == len(units) - 1)
                rsrc, Brsrc = (hmid, Bd["hmid"]) if ui == 0 else (hacc, Bd["hacc"])
                for cb in range(4):
                    s = cd % 2
                    cd += 1
                    S.dma("pool", I("dma_start", out=Wdn[s][:, 0:Fc, :], in_=dn[:, :, cb * 512:(cb + 1) * 512]), writes=[BWdn[s]])
                    for j in range(8):
                        bk = 4 + rot["t"] % 4
                        rot["t"] += 1
                        for f in range(Fc):
                            mm(bk, ps[bk][:, :], actT[:, f, j * 128:(j + 1) * 128], Wdn[s][:, f, :], f == 0, f == Fc - 1, [Bact[f], BWdn[s]])
                        r = (j + cb) % 2
                        S.dma("sp", I("dma_start", out=rb[r], in_=rsrc[j][:, cb * 512:(cb + 1) * 512]),
                              reads=[Brsrc[j][cb]], writes=[Brb[r]])
                        if ex is None:
                            S.op("dve", I("tensor_tensor", out=obf[r], in0=ps[bk][:, :], in1=rb[r], op=ALU.add),
                                 reads=[Bps[bk], Brb[r]], writes=[Bob[r]])
                        else:
                            S.op("dve", I("scalar_tensor_tensor", out=obf[r], in0=ps[bk][:, :], scalar=comb[:, j, ex:ex + 1], in1=rb[r],
                                                                                                 op0=ALU.mult, op1=ALU.add),
                                 reads=[Bps[bk], Brb[r], Bcomb[j]], writes=[Bob[r]])
                        if last:
                            S.dma("sp", I("dma_start", out=dst[dtile0 + j][:, cb * 512:(cb + 1) * 512], in_=obf[r]),
                                  reads=[Bob[r]], writes=[Bdst[dtile0 + j][cb]])
                        else:
                            S.dma("sp", I("dma_start", out=hacc[j][:, cb * 512:(cb + 1) * 512], in_=obf[r]),
                                  reads=[Bob[r]], writes=[Bd["hacc"][j][cb]])
            S.barrier()

        for (l, qoff, sn, dn_) in passes:
            layer_pass(l, qoff, sn, dn_)
        S.barrier()
        S.emit()
    return nc


def _consts(half):
    pos = np.zeros((128, 16), np.int64)
    for i in range(16):
        J = (i + 8 * half) % 16
        pos[:, i] = J * 128 + np.arange(128)
    freqs = (np.float32(10000.0) ** (np.float32(-2.0) * np.arange(32, dtype=np.float32) / np.float32(64))).astype(np.float32)
    ang = pos.astype(np.float32)[:, :, None] * freqs[None, None, :]
    cs = np.concatenate([np.cos(ang), np.sin(ang)], axis=-1).astype(np.float32)
    lsel = np.full((128, 2), NEG, np.float32)
    lsel[:, half] = 0.0
    ident = np.eye(128, dtype=np.float32)
    anti = np.eye(64, dtype=np.float32)[::-1].copy()
    qc = np.arange(64)
    cstart = np.clip(qc - 8, 0, 48)
    kc = np.arange(64)
    cm = ((kc[:, None] >= cstart[None, :]) & (kc[:, None] <= cstart[None, :] + 15)).astype(np.float32)
    colmask = np.concatenate([cm, cm], axis=0)
    k = np.arange(128)[:, None]
    q = np.arange(128)[None, :]
    trimask = np.stack([(k >= q), (k <= q)], axis=1).astype(np.float32)
    return dict(cs=cs, lsel=lsel, ident=ident, antiI=anti, colmask=colmask, trimask=trimask)


def _tile(v, n):
    return np.tile(np.asarray(v, np.float32), n)


def _layer_inputs(l, inp):
    d = {}
    d["w_in%d" % l] = inp["w_in"][l]
    d["uq%d" % l] = inp["mla_w_uq"][l]
    d["ukv%d" % l] = inp["mla_w_ukv"][l]
    d["mkv%d" % l] = inp["mem_w_kv"][l]
    d["wbr%d" % l] = inp["w_branch"][l]
    d["wg%d" % l] = inp["w_gate"][l]
    d["wo%d" % l] = inp["w_o"][l]
    mk = np.asarray(inp["mla_k_norm"][l], np.float32)
    gv = np.concatenate([
        inp["norm_mix"][l], _tile(inp["na_q_norm"][l], 4), _tile(inp["na_k_norm"][l], 4), inp["mla_cq_norm"][l],
        inp["mla_ckv_norm"][l], _tile(inp["mla_q_norm"][l], 4), _tile(mk[:128], 4), mk[128:],
        _tile(inp["swa_q_norm"][l], 8), _tile(inp["swa_k_norm"][l], 2), inp["mem_norm"][l],
        _tile(inp["mem_q_norm"][l], 4), _tile(inp["mem_k_norm"][l], 4), inp["norm_ffn"][l], inp["swa_sink"][l],
        np.zeros(56, np.float32)]).astype(np.float32)
    assert gv.shape[0] == NGV
    d["gv%d" % l] = gv[None, :]
    d["bgT%d" % l] = np.ascontiguousarray(np.asarray(inp["b_gate"][l], np.float32).reshape(4, 16, 128).transpose(2, 0, 1).reshape(128, 64))
    rp = np.zeros((60, 160), np.float32)
    r = np.asarray(inp["na_rpb"][l], np.float32)[:, ::-1, :].reshape(60, 31)
    rp[:, 48:48 + 31] = r
    d["rpbp%d" % l] = rp
    if l % 2 == 0:
        d["ffn_up"] = inp["ffn_w_up"][l // 2]
        d["ffn_down"] = inp["ffn_w_down"][l // 2]
    else:
        d["router"] = inp["moe_router"][l // 2]
        d["moe_up"] = inp["moe_w_up"][l // 2]
        d["moe_down"] = inp["moe_w_down"][l // 2]
    return d


def _perm_rows(hb, half):
    t = hb.reshape(16, 128, D)
    idx = [(i + 8 * half) % 16 for i in range(16)]
    return np.ascontiguousarray(t[idx])


_CACHE = {}


def _get_prog(key, passes, layers):
    if key not in _CACHE:
        _CACHE[key] = build(passes, layers, True)
    return _CACHE[key]


def _launch(nc, h_full, inp, layers):
    in_maps = []
    for c in range(8):
        b, half = c // 2, c % 2
        m = {"xin": _perm_rows(h_full[b], half), "mem": np.ascontiguousarray(np.asarray(inp["mem"][b], np.float32).reshape(2, 128, D))}
        m.update(_consts(half))
        for l in layers:
            m.update(_layer_inputs(l, inp))
        in_maps.append(m)
    names = set()
    for alloc in nc.allocations:
        if isinstance(alloc, mybir.MemoryLocationSet) and alloc.kind == "ExternalInput":
            names.add(alloc.memorylocations[0].name)
    in_maps = [{k: v for k, v in m.items() if k in names} for m in in_maps]
    res = run_bass_kernel_spmd(nc, in_maps, core_ids=list(range(8)))
    out = np.zeros((4, 2048, D), np.float32)
    for c in range(8):
        b, half = c // 2, c % 2
        out[b, half * 1024:(half + 1) * 1024] = res.results[c]["hout"].reshape(1024, D)
    return out, res


def kernel(**inp):
    inp = {k: np.asarray(v) for k, v in inp.items()}
    x = np.asarray(inp["x"], np.float32)
    if FUSED:
        nc = _get_prog("fused", [(0, 0, "xin", "h1s"), (0, 8, "xin", "h1s"), (1, 0, "h1s", "hout")], [0, 1])
        out, _ = _launch(nc, x, inp, [0, 1])
        return out
    nc0 = _get_prog("l0", [(0, 0, "xin", "hout")], [0])
    h1, _ = _launch(nc0, x, inp, [0])
    nc1 = _get_prog("l1", [(1, 0, "xin", "hout")], [1])
    out, _ = _launch(nc1, h1, inp, [1])
    return out
```

```python
import contextlib
import numpy as np
import concourse.bass as bass
import concourse.mybir as mybir
from concourse.bass_utils import run_bass_kernel_spmd

F32 = mybir.dt.float32
BF16 = mybir.dt.bfloat16
AF = mybir.ActivationFunctionType
ALU = mybir.AluOpType
AX = mybir.AxisListType

D = 2048
EPS = 1e-6
NEG = -30000.0
FUSED = True
DEBUG = False
STOP = None
CLEAR_PROTO = False
USE_INTERNAL = True
STAGES = ["U", "NA", "MLA", "SWA", "MEM", "MERGE", "FFN"]


class Buf:
    __slots__ = ("name", "w", "r", "dsem", "gen")

    def __init__(self, name=""):
        self.name = name
        self.w = None
        self.r = []
        self.dsem = None
        self.gen = -1


class Sched:
    ENGS = ("pe", "act", "dve", "pool", "sp")

    def __init__(self, nc):
        self.nc = nc
        self.ops = {e: [] for e in self.ENGS}
        self.nops = {e: 0 for e in self.ENGS}
        self.seen = {e: {} for e in self.ENGS}
        self.signal = {e: set() for e in self.ENGS}
        self.n_sem = {"s": 0, "p": 0}
        self.sem_total = {"s": [], "p": []}
        self.next_free = {"s": 0, "p": 0}
        self.gen = 0
        self.pending_dma = []

    def _need(self, eng, tok, waits):
        if tok is None:
            return
        if tok[0] == "c":
            _, te, idx = tok
            if te == eng and eng in ("pe", "sp"):
                return
            key = ("c", te)
            if self.seen[eng].get(key, -1) >= idx:
                return
            self.seen[eng][key] = idx
            waits.append(tok)
            self.signal[te].add(idx)
        else:
            _, sem, val, g = tok
            key = ("d", sem)
            if self.seen[eng].get(key, -1) >= val:
                return
            self.seen[eng][key] = val
            waits.append(tok)

    def _deps(self, eng, reads, writes):
        waits = []
        for b in reads:
            self._need(eng, b.w, waits)
        for b in writes:
            self._need(eng, b.w, waits)
            for t in b.r:
                self._need(eng, t, waits)
        return waits

    def op(self, eng, fn, reads=(), writes=()):
        waits = self._deps(eng, reads, writes)
        idx = self.nops[eng]
        self.nops[eng] += 1
        tok = ("c", eng, idx)
        self.ops[eng].append(("op", fn, waits, idx))
        for b in reads:
            b.r.append(tok)
        for b in writes:
            b.w = tok
            b.r = []
        return tok

    def dma(self, eng, fn, reads=(), writes=()):
        waits = self._deps(eng, reads, writes)
        db = writes[0]
        cl = "p" if eng == "pool" else "s"
        if db.dsem is None or db.gen != self.gen or db.dsem[0] != cl:
            if self.next_free[cl] >= self.n_sem[cl]:
                self.n_sem[cl] += 1
                self.sem_total[cl].append(0)
            db.dsem = (cl, self.next_free[cl])
            self.next_free[cl] += 1
            db.gen = self.gen
        self.sem_total[cl][db.dsem[1]] += 16
        tok = ("d", db.dsem, self.sem_total[cl][db.dsem[1]], self.gen)
        self.ops[eng].append(("dma", fn, waits, db.dsem))
        self.pending_dma.append(tok)
        for b in reads:
            b.r.append(tok)
        for b in writes:
            b.w = tok
            b.r = []
        return tok

    def barrier(self):
        last = {}
        for e in self.ENGS:
            if self.nops[e] > 0:
                last[e] = ("c", e, self.nops[e] - 1)
        dm = {}
        for t in self.pending_dma:
            dm[t[1]] = max(dm.get(t[1], 0), t[2])
        self.pending_dma = []
        for e in self.ENGS:
            waits = []
            for te, tok in last.items():
                if te != e or e not in ("pe", "sp"):
                    self._need(e, tok, waits)
            for sem, val in dm.items():
                self._need(e, ("d", sem, val, self.gen), waits)
            self.ops[e].append(("wait", None, waits, None))
        npool = self.next_free["p"]
        if npool > 0 and CLEAR_PROTO:
            toks = [self.op(e, I("nop")) for e in ("pe", "act", "dve", "sp")]
            waits = []
            for t in toks:
                self._need("pool", t, waits)
            self.ops["pool"].append(("clear", None, waits, npool))
            tp = self.op("pool", I("nop"))
            for e in ("pe", "act", "dve", "sp"):
                waits = []
                self._need(e, tp, waits)
                self.ops[e].append(("wait", None, waits, None))
            for i in range(npool):
                self.sem_total["p"][i] = 0
            for e in self.ENGS:
                for i in range(npool):
                    self.seen[e].pop(("d", ("p", i)), None)
        self.gen += 1
        self.next_free = {"s": 0, "p": 0}

    def emit(self):
        nc = self.nc
        stack = contextlib.ExitStack()
        with stack:
            csem = {e: stack.enter_context(nc.semaphore("cs_" + e)) for e in self.ENGS}
            dsem = {(cl, i): stack.enter_context(nc.semaphore("ds%s%d" % (cl, i))) for cl in ("s", "p") for i in range(self.n_sem[cl])}
            print("semaphores: sp-dma %d pool-dma %d" % (self.n_sem["s"], self.n_sem["p"]))
            rank = {}
            for e in self.ENGS:
                for r, idx in enumerate(sorted(self.signal[e])):
                    rank[(e, idx)] = r + 1
            block = stack.enter_context(nc.Block())
            handles = {"pe": block.tensor, "act": block.scalar, "dve": block.vector,
                       "pool": block.gpsimd, "sp": block.sync}

            used_p = set()

            def make(e):
                def body(eng):
                    for kind, fn, waits, extra in self.ops[e]:
                        for t in waits:
                            if t[0] == "c":
                                eng.wait_ge(csem[t[1]], rank[(t[1], t[2])])
                            else:
                                eng.wait_ge(dsem[t[1]], t[2])
                        if kind == "op":
                            ins = getattr(eng, fn[0])(*fn[1], **fn[2])
                            if (e, extra) in rank:
                                ins.then_inc(csem[e], 1)
                        elif kind == "clear":
                            if CLEAR_PROTO != "nop":
                                for i in range(extra):
                                    eng.sem_clear(dsem[("p", i)])
                        elif kind == "dma":
                            getattr(eng, fn[0])(*fn[1], **fn[2]).then_inc(dsem[extra], 16)
                return body

            for e in self.ENGS:
                handles[e](make(e))


class Arena:
    def __init__(self, t, nbytes):
        self.t = t
        self.n = nbytes
        self.top = 0
        self.peak = 0

    def alloc(self, shape, dt):
        es = 4 if dt == F32 else 2
        n = int(np.prod(shape))
        nb = (n * es + 31) // 32 * 32
        off = self.top
        self.top += nb
        self.peak = max(self.peak, self.top)
        assert self.top <= self.n, "arena overflow %d > %d" % (self.top, self.n)
        v = self.t[:, off // 2: off // 2 + (n * es) // 2]
        if dt == F32:
            v = v.bitcast(F32)
        if len(shape) == 2:
            v = v.rearrange("p (a b) -> p a b", a=shape[0])
        elif len(shape) == 3:
            v = v.rearrange("p (a b c) -> p a b c", a=shape[0], b=shape[1])
        elif len(shape) == 4:
            v = v.rearrange("p (a b c d) -> p a b c d", a=shape[0], b=shape[1], c=shape[2])
        return v

    def mark(self):
        return self.top

    def release(self, m):
        self.top = m


def I(name, *args, **kw):
    return (name, args, kw)


def bcast(ap, shape):
    return ap.unsqueeze(len(ap.shape)).to_broadcast(list(shape))


def bc_mid(ap, n):
    a = [list(x) for x in ap.ap]
    return bass.AP(ap.tensor, ap.offset, [a[0], [0, n]] + a[1:])


GSEC = {}
_o = 0
for _n, _s in [("mix", 2048), ("naq", 512), ("nak", 512), ("cq", 768), ("ckv", 256), ("mq", 768),
               ("mkn", 512), ("mkr", 64), ("sq", 512), ("sk", 128), ("memn", 2048), ("memq", 512),
               ("memk", 512), ("ffn", 2048), ("sink", 64)]:
    GSEC[_n] = (_o, _s)
    _o += _s
NGV = _o


def build(passes, layers, use_moe):
    nc = bass.Bass("TRN2", target_bir_lowering=False)

    def din(name, shape):
        return nc.dram_tensor(name, list(shape), F32, kind="ExternalInput").ap()

    xin = din("xin", [16, 128, D])
    memd = din("mem", [2, 128, D])
    csd = din("cs", [128, 16, 64])
    lseld = din("lsel", [128, 2])
    identd = din("ident", [128, 128])
    antid = din("antiI", [64, 64])
    colmd = din("colmask", [128, 64])
    trimd = din("trimask", [128, 2, 128])
    Wd = {}
    sidx = STAGES.index(STOP) if STOP else 6
    for l in layers:
        w = {}
        w["gv"] = din("gv%d" % l, [1, NGV])
        if sidx >= 1:
            w["w_in"] = din("w_in%d" % l, [2048, 3904])
            w["rpbp"] = din("rpbp%d" % l, [60, 160])
        if sidx >= 2:
            w["uq"] = din("uq%d" % l, [768, 768])
            w["ukv"] = din("ukv%d" % l, [256, 1024])
        if sidx >= 4:
            w["mkv"] = din("mkv%d" % l, [2048, 1024])
        if sidx >= 5:
            w["wbr"] = din("wbr%d" % l, [4, 512, 2048])
            w["wg"] = din("wg%d" % l, [2048, 4, 2048])
            w["wo"] = din("wo%d" % l, [2048, 2048])
            w["bgT"] = din("bgT%d" % l, [128, 64])
        if sidx < 6:
            pass
        elif l % 2 == 0:
            w["up"] = din("ffn_up", [2048, 11264])
            w["down"] = din("ffn_down", [5632, 2048])
        else:
            w["router"] = din("router", [2048, 8])
            w["mup"] = din("moe_up", [8, 2048, 14336])
            w["mdown"] = din("moe_down", [8, 7168, 2048])
        Wd[l] = w
    hout = nc.dram_tensor("hout", [8, 128, D], F32, kind="ExternalOutput").ap()
    h1s = nc.dram_tensor("h1s", [16, 128, D], F32, kind="Internal").ap()
    hmid = nc.dram_tensor("hmid", [8, 128, D], F32, kind="Internal").ap()
    hacc = nc.dram_tensor("hacc", [8, 128, D], F32, kind="Internal").ap()
    dbg = None
    if DEBUG:
        dbg = nc.dram_tensor("dbg", [8, 128, D], F32, kind="ExternalOutput").ap()
    dram = {"xin": xin, "h1s": h1s, "hout": hout}
    Bd = {k: [[Buf()] * 4 for _ in range(16)] for k in ("xin", "h1s", "hout", "hmid", "hacc")}

    st = contextlib.ExitStack()
    with st:
        def sb(name, shape, dt):
            return st.enter_context(nc.sbuf_tensor(name, list(shape), dt))
        ARENA = 196 * 1024
        arena_t = sb("arena", [128, ARENA // 2], BF16)
        identf = sb("identf", [128, 128], F32)
        identb = sb("identb", [128, 128], BF16)
        anti = sb("anti", [64, 64], F32)
        colm = sb("colm", [128, 64], F32)
        trimf = sb("trimf", [128, 2, 128], F32)
        trim = sb("trim", [128, 2, 128], BF16)
        cst = sb("cst", [128, 16, 64], F32)
        lsel = sb("lselt", [128, 2], F32)
        ps = [st.enter_context(nc.psum_tensor("ps%d" % i, [128, 512], F32)) for i in range(8)]
        Bps = [Buf("ps%d" % i) for i in range(8)]
        S = Sched(nc)
        ar = Arena(arena_t, ARENA)
        Bconst = Buf("const")

        for dst, src in ((identf, identd), (anti, antid), (colm, colmd), (trimf, trimd), (cst, csd), (lsel, lseld)):
            S.dma("sp", I("dma_start", out=dst[:], in_=src), writes=[Buf()])
        S.barrier()
        S.op("dve", I("tensor_copy", out=identb[:], in_=identf[:]), writes=[Bconst])
        S.op("dve", I("tensor_copy", out=trim[:], in_=trimf[:]), writes=[Bconst])
        S.barrier()

        rot = {"t": 0}

        def psT(i):
            return ps[i][:, :].bitcast(BF16)

        def mm(bank, out_ap, lhsT, rhs, start, stop, reads):
            S.op("pe", I("matmul", out_ap, lhsT=lhsT, rhs=rhs, start=start, stop=stop, skip_group_check=True),
                 reads=reads, writes=[Bps[bank]])

        def wload(dst_ap, src_ap, buf):
            S.dma("pool", I("dma_start", out=dst_ap, in_=src_ap), writes=[buf])

        def bload(dst_ap, gv, sec, buf):
            o, n = GSEC[sec]
            S.dma("sp", I("dma_start", out=dst_ap, in_=gv[0:1, o:o + n].partition_broadcast(128)), writes=[buf])

        def rstd_of(ss_ap, n, dim, reads, wbuf):
            t = ar.alloc([n], F32)
            rs = ar.alloc([n], F32)
            bt = Buf()
            S.op("act", I("activation", out=t, in_=ss_ap, func=AF.Sqrt, bias=EPS, scale=1.0 / dim),
                 reads=reads, writes=[bt])
            S.op("dve", I("reciprocal", out=rs, in_=t), reads=[bt], writes=[wbuf])
            return rs

        def headnorm(zs3, Bzs, H, d, gain2, Bg, out3, Bout, sq_scr):
            n = H * d
            sq = sq_scr[:, 0:n]
            Bsq = Buf()
            S.op("dve", I("tensor_tensor", out=sq, in0=zs3.rearrange("p h d -> p (h d)"), in1=zs3.rearrange("p h d -> p (h d)"), op=ALU.mult),
                 reads=[Bzs], writes=[Bsq])
            ss = ar.alloc([H], F32)
            Bss = Buf()
            S.op("dve", I("tensor_reduce", out=ss, in_=sq.rearrange("p (h d) -> p h d", h=H), axis=AX.X, op=ALU.add),
                 reads=[Bsq], writes=[Bss])
            Brs = Buf()
            rs = rstd_of(ss, H, d, [Bss], Brs)
            S.op("dve", I("tensor_tensor", out=sq.rearrange("p (h d) -> p h d", h=H), in0=zs3, in1=bcast(rs, [128, H, d]), op=ALU.mult),
                 reads=[Bzs, Brs], writes=[Bsq])
            S.op("dve", I("tensor_tensor", out=out3.rearrange("p h d -> p (h d)"), in0=sq, in1=gain2, op=ALU.mult),
                 reads=[Bsq, Bg], writes=[Bout])
            return rs, Brs

        def rope(x3, Bx, H, ti, out3, Bout, scr):
            cos = bc_mid(cst[:, ti, 0:32], H)
            sin = bc_mid(cst[:, ti, 32:64], H)
            a = scr[:, 0:H * 32].rearrange("p (h d) -> p h d", h=H)
            b = scr[:, H * 32:H * 64].rearrange("p (h d) -> p h d", h=H)
            x1 = x3[:, :, 0:32]
            x2 = x3[:, :, 32:64]
            Bs = Buf()
            S.op("dve", I("tensor_tensor", out=a, in0=x1, in1=cos, op=ALU.mult), reads=[Bx], writes=[Bs])
            S.op("dve", I("tensor_tensor", out=b, in0=x2, in1=sin, op=ALU.mult), reads=[Bx], writes=[Bs])
            S.op("dve", I("tensor_tensor", out=out3[:, :, 0:32], in0=a, in1=b, op=ALU.subtract), reads=[Bs], writes=[Bout])
            S.op("dve", I("tensor_tensor", out=a, in0=x2, in1=cos, op=ALU.mult), reads=[Bx], writes=[Bs])
            S.op("dve", I("tensor_tensor", out=b, in0=x1, in1=sin, op=ALU.mult), reads=[Bx], writes=[Bs])
            S.op("dve", I("tensor_tensor", out=out3[:, :, 32:64], in0=a, in1=b, op=ALU.add), reads=[Bs], writes=[Bout])

        TB = (6, 7)

        def transposes(srcs, Bsrc, dst_fn, Bdst, nrows=128):
            g = 0
            while g < len(srcs):
                n = min(8, len(srcs) - g)
                bk = TB[rot["t"] % 2]
                rot["t"] += 1
                pt = psT(bk)
                for q in range(n):
                    S.op("pe", I("transpose", out=pt[0:nrows, q * 128:(q + 1) * 128], in_=srcs[g + q], identity=identb[:]),
                         reads=[Bsrc, Bconst], writes=[Bps[bk]])
                dst = dst_fn(g, n)
                S.op("act", I("copy", out=dst, in_=pt[0:nrows, 0:n * 128].rearrange("p (n t) -> p n t", n=n)),
                     reads=[Bps[bk]], writes=[Bdst])
                g += n

        def rmsnorm_tile(src_tile_ap, src_bufs, gain, Bgain, hl, Bhl, ub, Bub):
            S.dma("sp", I("dma_start", out=hl, in_=src_tile_ap), reads=src_bufs, writes=[Bhl])
            ss = ar.alloc([1], F32)
            Bss = Buf()
            S.op("act", I("activation", out=ub, in_=hl, func=AF.Square, accum_out=ss),
                 reads=[Bhl], writes=[Bub, Bss])
            Brs = Buf()
            rs = rstd_of(ss, 1, D, [Bss], Brs)
            S.op("dve", I("scalar_tensor_tensor", out=ub, in0=hl, scalar=rs[:, 0:1], in1=gain, op0=ALU.mult, op1=ALU.mult),
                 reads=[Bhl, Brs, Bgain], writes=[Bub])
            return rs, Brs

        def layer_pass(l, qoff, src_name, dst_name):
            W = Wd[l]
            src = dram[src_name]
            Bsrc = Bd[src_name]
            gv = W["gv"]
            S.barrier()
            ar.release(0)
            uT = ar.alloc([16, 2048], BF16)
            B_uT = [Buf() for _ in range(16)]
            br = ar.alloc([8, 2048], BF16)
            B_br = [Buf() for _ in range(8)]
            if STOP:
                S.op("dve", I("memset", br, 0.0), writes=B_br)
            base = ar.mark()

            gmix = ar.alloc([2048], F32)
            Bg = Buf()
            bload(gmix, gv, "mix", Bg)
            hl = [ar.alloc([2048], F32) for _ in range(2)]
            Bhl = [Buf(), Buf()]
            ub = [ar.alloc([2048], BF16) for _ in range(2)]
            Bub = [Buf(), Buf()]
            for i in range(16):
                s = i % 2
                rmsnorm_tile(src[i], Bsrc[i], gmix, Bg, hl[s], Bhl[s], ub[s], Bub[s])
                transposes([ub[s][:, k * 128:(k + 1) * 128] for k in range(16)], Bub[s],
                           lambda g0, n, i=i: uT[:, g0:g0 + n, i * 128:(i + 1) * 128], B_uT[i])
            S.barrier()
            ar.release(base)

            def stop_here(stage):
                if STOP != stage:
                    return False
                if stage == "MERGE":
                    S.dma("sp", I("dma_start", out=hout, in_=hmid), reads=[Bd["hmid"][j][0] for j in range(8)], writes=[Buf()])
                else:
                    srcv = uT[:, 0:8, :] if stage == "U" else br
                    S.dma("pool", I("dma_start", out=hout.rearrange("t p d -> p t d"), in_=srcv), writes=[Buf()])
                S.barrier()
                return True

            if stop_here("U"):
                return
            w_in = W["w_in"].rearrange("(k p) n -> p k n", p=128)

            def zproj(i, wt, Bw, ncols, bank):
                for k in range(16):
                    mm(bank, ps[bank][:, 0:ncols], uT[:, k, i * 128:(i + 1) * 128], wt[:, k, 0:ncols], k == 0, k == 15,
                       [B_uT[i], Bw])

            ZB = (0, 1)

            def attend(j, visits, nheads, dv, score_mms, exp_scale, bias_of, mask_fn, v_of, hstride, heads_per_bank,
                       PTb, BPT, out_fn):
                OB = (4, 5)
                nv = len(visits)
                for vi, v in enumerate(visits):
                    sbk = 2 + (rot["t"] % 2)
                    rot["t"] += 1
                    first = True
                    for h in range(nheads):
                        ml = score_mms(v, h)
                        for mi, (lt, rh, rd) in enumerate(ml):
                            mm(sbk, ps[sbk][:, h * 128:(h + 1) * 128], lt, rh, first, (h == nheads - 1 and mi == len(ml) - 1), rd)
                            first = False
                    pi = vi % 2
                    PT = PTb[pi]
                    bias = bias_of(v)
                    pin = ps[sbk][:, 0:nheads * 128].rearrange("p (h q) -> p h q", h=nheads)
                    if bias is None:
                        S.op("act", I("activation", out=PT, in_=pin, func=AF.Exp, scale=exp_scale),
                             reads=[Bps[sbk]], writes=[BPT[pi]])
                    else:
                        S.op("act", I("activation", out=PT, in_=pin, func=AF.Exp, bias=bias, scale=exp_scale),
                             reads=[Bps[sbk], Bconst], writes=[BPT[pi]])
                    if mask_fn is not None:
                        mask_fn(v, PT, BPT[pi])
                    for h in range(nheads):
                        ob = OB[h // heads_per_bank]
                        hh = h % heads_per_bank
                        vr, vreads = v_of(v, h)
                        mm(ob, ps[ob][:, hh * hstride:hh * hstride + dv + 1], PT[:, h, :], vr,
                           (vi == 0 and hh == 0), (vi == nv - 1), [BPT[pi]] + vreads)
                out_fn(OB)

            def finish_plain(j, nheads, dv, hstride, heads_per_bank, col0, extra_den=None):
                def fn(OB):
                    for bi in range((nheads + heads_per_bank - 1) // heads_per_bank):
                        ob = OB[bi]
                        nh = min(heads_per_bank, nheads - bi * heads_per_bank)
                        rec = ar.alloc([nh], F32)
                        Brec = Buf()
                        denv = ps[ob][:, dv:dv + (nh - 1) * hstride + 1:hstride]
                        if extra_den is None:
                            S.op("dve", I("reciprocal", out=rec, in_=denv), reads=[Bps[ob]], writes=[Brec])
                        else:
                            ex, Bex = extra_den
                            h0 = bi * heads_per_bank
                            S.op("dve", I("tensor_tensor", out=rec, in0=denv, in1=ex[:, h0:h0 + nh], op=ALU.add),
                                 reads=[Bps[ob], Bex], writes=[Brec])
                            S.op("dve", I("reciprocal", out=rec, in_=rec), reads=[Brec], writes=[Brec])
                        for hh in range(nh):
                            h = bi * heads_per_bank + hh
                            S.op("dve", I("tensor_scalar",
                                out=br[:, j, col0 + h * dv:col0 + (h + 1) * dv], in0=ps[ob][:, hh * hstride:hh * hstride + dv],
                                scalar1=rec[:, hh:hh + 1], scalar2=None, op0=ALU.mult),
                                reads=[Bps[ob], Brec], writes=[B_br[j]])
                return fn

            def zstage(n=1024):
                zs = [ar.alloc([n], F32) for _ in range(2)]
                return zs, [Buf(), Buf()]

            m0 = ar.mark()
            KT = ar.alloc([4, 2048], BF16)
            BKT = [Buf() for _ in range(16)]
            Vx = ar.alloc([16, 4, 130], BF16)
            BV = [Buf() for _ in range(16)]
            QT = ar.alloc([4, 1024], BF16)
            BQT = [Buf() for _ in range(8)]
            Mf = ar.alloc([60, 64], BF16)
            BMf = Buf()
            gq = ar.alloc([512], F32)
            gk = ar.alloc([512], F32)
            Bgq, Bgk = Buf(), Buf()
            bload(gq, gv, "naq", Bgq)
            bload(gk, gv, "nak", Bgk)
            S.op("dve", I("memset", Vx[:, :, :, 128:130], 1.0), writes=BV)
            mE = ar.mark()
            G2 = ar.alloc([15, 2, 64], F32)
            BG2 = Buf()
            et = ar.alloc([512], F32)
            Bet = Buf()
            rp = W["rpbp"]
            for hgrp in range(4):
                for a in range(2):
                    srcap = bass.AP(rp.tensor, rp.offset + hgrp * 15 * 160, [[1, 64], [160, 15], [1, 64]])
                    S.dma("sp", I("dma_start", out=G2[0:64, :, a, :], in_=srcap), writes=[BG2])
                for g0 in (0, 8):
                    n = min(8, 15 - g0)
                    bk = ZB[rot["t"] % 2]
                    rot["t"] += 1
                    for q in range(n):
                        mm(bk, ps[bk][:, q * 64:(q + 1) * 64], G2[0:64, g0 + q, :, :].rearrange("p a k -> p (a k)"), anti[:, :],
                           True, True, [BG2, Bconst])
                    S.op("act", I("activation", out=et[:, 0:n * 64], in_=ps[bk][:, 0:n * 64], func=AF.Exp),
                         reads=[Bps[bk]], writes=[Bet])
                    S.op("dve", I("tensor_tensor",
                        out=Mf[:, hgrp * 15 + g0:hgrp * 15 + g0 + n, :], in0=et[:, 0:n * 64].rearrange("p (n q) -> p n q", n=n),
                        in1=bc_mid(colm[:, :], n), op=ALU.mult), reads=[Bet, Bconst], writes=[BMf])

            S.barrier()
            ar.release(mE)
            Wc = [ar.alloc([16, 512], BF16) for _ in range(2)]
            BWc = [Buf(), Buf()]
            zs, Bzs = zstage(512)
            sq_scr = ar.alloc([512], F32)
            xb = [ar.alloc([512], BF16) for _ in range(2)]
            Bxb = [Buf(), Buf()]
            def na_cols(cb, tiles, kind):
                s = cb % 2
                wload(Wc[s], w_in[:, :, cb * 512:(cb + 1) * 512], BWc[s])
                for i in tiles:
                    bk = ZB[rot["t"] % 2]
                    rot["t"] += 1
                    zproj(i, Wc[s], BWc[s], 512, bk)
                    z = i % 2
                    if kind == "v":
                        S.op("act", I("copy", out=Vx[:, i, :, 0:128], in_=ps[bk][:, :].rearrange("p (h d) -> p h d", h=4)),
                             reads=[Bps[bk]], writes=[BV[i]])
                        continue
                    S.op("act", I("copy", out=zs[z][:, 0:512], in_=ps[bk][:, :]), reads=[Bps[bk]], writes=[Bzs[z]])
                    z3 = zs[z][:, 0:512].rearrange("p (h d) -> p h d", h=4)
                    o3 = xb[z][:, 0:512].rearrange("p (h d) -> p h d", h=4)
                    if kind == "q":
                        headnorm(z3, Bzs[z], 4, 128, gq, Bgq, o3, Bxb[z], sq_scr)
                        j = i - qoff
                        transposes([xb[z][:, h * 128:(h + 1) * 128] for h in range(4)], Bxb[z],
                                   lambda g0, n, j=j: QT[:, g0:g0 + n, j * 128:(j + 1) * 128], BQT[j])
                    else:
                        headnorm(z3, Bzs[z], 4, 128, gk, Bgk, o3, Bxb[z], sq_scr)
                        transposes([xb[z][:, h * 128:(h + 1) * 128] for h in range(4)], Bxb[z],
                                   lambda g0, n, i=i: KT[:, g0:g0 + n, i * 128:(i + 1) * 128], BKT[i])

            qtiles = list(range(qoff, qoff + 8))
            na_cols(1, range(16), "k")
            na_cols(2, range(16), "v")
            na_cols(0, qtiles, "q")
            PTb = [ar.alloc([4, 128], BF16) for _ in range(2)]
            BPT = [Buf(), Buf()]

            def rs_(r):
                return min(max(r - 4, 0), 24)

            for j in range(8):
                i = qoff + j
                visits = []
                for hc in range(2):
                    J = (i + 8 * hc) % 16
                    for c in range(16):
                        val = [[rs_(2 * J + b) <= 2 * c + a <= rs_(2 * J + b) + 7 for b in range(2)] for a in range(2)]
                        if any(val[0]) or any(val[1]):
                            visits.append((hc, J, c, (c + 8 * hc) % 16, val))

                def na_scores(v, h, j=j):
                    lc = v[3]
                    return [(KT[:, h, lc * 128:(lc + 1) * 128], QT[:, h, j * 128:(j + 1) * 128], [BKT[lc], BQT[j]])]

                def na_mask(v, PT, BP):
                    hc, J, c, lc, val = v
                    for a in range(2):
                        pa = slice(a * 64, (a + 1) * 64)
                        drp0 = 7 - 2 * c - a + 2 * J
                        if val[a][0] and val[a][1]:
                            mv = bass.AP(Mf.tensor, Mf[pa, drp0, 0:1].offset, [list(Mf.ap[0])[0:1] + [64], [15 * 64, 4], [1, 128]])
                            S.op("dve", I("tensor_tensor", out=PT[pa, :, :], in0=PT[pa, :, :], in1=mv, op=ALU.mult),
                                 reads=[BP, BMf], writes=[BP])
                        else:
                            for b in range(2):
                                qs = slice(b * 64, (b + 1) * 64)
                                if val[a][b]:
                                    mv = bass.AP(Mf.tensor, Mf[pa, drp0 + b, 0:1].offset, [list(Mf.ap[0])[0:1] + [64], [15 * 64, 4], [1, 64]])
                                    S.op("dve", I("tensor_tensor", out=PT[pa, :, qs], in0=PT[pa, :, qs], in1=mv, op=ALU.mult),
                                         reads=[BP, BMf], writes=[BP])
                                else:
                                    S.op("dve", I("memset", PT[pa, :, qs], 0.0), reads=[BP], writes=[BP])

                attend(j, visits, 4, 128, na_scores, 128 ** -0.5, lambda v: lsel[:, v[0]:v[0] + 1], na_mask,
                       lambda v, h: (Vx[:, v[3], h, 0:129], [BV[v[3]]]), 130, 2, PTb, BPT,
                       finish_plain(j, 4, 128, 130, 2, 0))
            S.barrier()
            ar.release(m0)
            if stop_here("NA"):
                return

            QTn = ar.alloc([4, 1024], BF16)
            QTr = ar.alloc([2, 1024], BF16)
            BQT = [Buf() for _ in range(8)]
            m1 = ar.mark()
            Wq = ar.alloc([16, 768], BF16)
            BWq = Buf()
            wload(Wq, w_in[:, :, 1536:2304], BWq)
            Wuq = ar.alloc([6, 768], BF16)
            BWuq = Buf()
            wload(Wuq, W["uq"].rearrange("(k p) n -> p k n", p=128), BWuq)
            gcq = ar.alloc([768], F32)
            gmq = ar.alloc([768], F32)
            Bgcq, Bgmq = Buf(), Buf()
            bload(gcq, gv, "cq", Bgcq)
            bload(gmq, gv, "mq", Bgmq)
            zs, Bzs = zstage(768)
            sq_scr = ar.alloc([768], F32)
            xb = [ar.alloc([1024], BF16) for _ in range(2)]
            Bxb = [Buf(), Buf()]
            cqT = [ar.alloc([6, 128], BF16) for _ in range(2)]
            BcqT = [Buf(), Buf()]
            qn = [ar.alloc([768], F32) for _ in range(2)]
            Bqn = [Buf(), Buf()]
            qr = [ar.alloc([256], F32) for _ in range(2)]
            Bqr = [Buf(), Buf()]
            for j in range(8):
                i = qoff + j
                z = j % 2
                b0, b1 = ZB
                for k in range(16):
                    mm(b0, ps[b0][:, :], uT[:, k, i * 128:(i + 1) * 128], Wq[:, k, 0:512], k == 0, k == 15, [B_uT[i], BWq])
                for k in range(16):
                    mm(b1, ps[b1][:, 0:256], uT[:, k, i * 128:(i + 1) * 128], Wq[:, k, 512:768], k == 0, k == 15, [B_uT[i], BWq])
                S.op("act", I("copy", out=zs[z][:, 0:512], in_=ps[b0][:, :]), reads=[Bps[b0]], writes=[Bzs[z]])
                S.op("act", I("copy", out=zs[z][:, 512:768], in_=ps[b1][:, 0:256]), reads=[Bps[b1]], writes=[Bzs[z]])
                headnorm(zs[z][:, 0:768].rearrange("p (h d) -> p h d", h=1), Bzs[z], 1, 768, gcq, Bgcq,
                         xb[z][:, 0:768].rearrange("p (h d) -> p h d", h=1), Bxb[z], sq_scr)
                transposes([xb[z][:, c * 128:(c + 1) * 128] for c in range(6)], Bxb[z],
                           lambda g0, n, z=z: cqT[z][:, g0:g0 + n, :], BcqT[z])
                for c in range(6):
                    mm(b0, ps[b0][:, :], cqT[z][:, c, :], Wuq[:, c, 0:512], c == 0, c == 5, [BcqT[z], BWuq])
                for c in range(6):
                    mm(b1, ps[b1][:, 0:256], cqT[z][:, c, :], Wuq[:, c, 512:768], c == 0, c == 5, [BcqT[z], BWuq])
                S.op("act", I("copy", out=zs[z][:, 0:512], in_=ps[b0][:, :]), reads=[Bps[b0]], writes=[Bzs[z]])
                S.op("act", I("copy", out=zs[z][:, 512:768], in_=ps[b1][:, 0:256]), reads=[Bps[b1]], writes=[Bzs[z]])
                q3 = qn[z][:, :].rearrange("p (h d) -> p h d", h=4)
                headnorm(zs[z][:, 0:768].rearrange("p (h d) -> p h d", h=4), Bzs[z], 4, 192, gmq, Bgmq, q3, Bqn[z], sq_scr)
                S.op("dve", I("tensor_copy", out=xb[z][:, 0:512].rearrange("p (h d) -> p h d", h=4), in_=q3[:, :, 0:128]),
                     reads=[Bqn[z]], writes=[Bxb[z]])
                S.op("dve", I("tensor_copy", out=qr[z][:, :].rearrange("p (h d) -> p h d", h=4), in_=q3[:, :, 128:192]),
                     reads=[Bqn[z]], writes=[Bqr[z]])
                rope(qr[z][:, :].rearrange("p (h d) -> p h d", h=4), Bqr[z], 4, i,
                     xb[z][:, 512:768].rearrange("p (h d) -> p h d", h=4), Bxb[z], sq_scr)
                transposes([xb[z][:, h * 128:(h + 1) * 128] for h in range(4)], Bxb[z],
                           lambda g0, n, j=j: QTn[:, g0:g0 + n, j * 128:(j + 1) * 128], BQT[j])
                transposes([xb[z][:, 512 + p * 128:512 + (p + 1) * 128] for p in range(2)], Bxb[z],
                           lambda g0, n, j=j: QTr[:, g0:g0 + n, j * 128:(j + 1) * 128], BQT[j])
            S.barrier()
            ar.release(m1)
            KTn = ar.alloc([4, 2048], BF16)
            KTr = ar.alloc([2, 2048], BF16)
            BKT = [Buf() for _ in range(16)]
            Vx = ar.alloc([16, 4, 130], BF16)
            BV = [Buf() for _ in range(16)]
            S.op("dve", I("memset", Vx[:, :, :, 128:130], 1.0), writes=BV)
            m1 = ar.mark()
            Wk = ar.alloc([16, 320], BF16)
            BWk = Buf()
            wload(Wk, w_in[:, :, 2304:2624], BWk)
            Wukv = ar.alloc([2, 1024], BF16)
            BWukv = Buf()
            wload(Wukv, W["ukv"].rearrange("(k p) n -> p k n", p=128), BWukv)
            gckv = ar.alloc([256], F32)
            gkn = ar.alloc([512], F32)
            gkr = ar.alloc([64], F32)
            Bgc, Bgn, Bgr = Buf(), Buf(), Buf()
            bload(gckv, gv, "ckv", Bgc)
            bload(gkn, gv, "mkn", Bgn)
            bload(gkr, gv, "mkr", Bgr)
            zs, Bzs = zstage(320)
            sq_scr = ar.alloc([768], F32)
            xb = [ar.alloc([1024], BF16) for _ in range(2)]
            Bxb = [Buf(), Buf()]
            ckT = [ar.alloc([2, 128], BF16) for _ in range(2)]
            BckT = [Buf(), Buf()]
            kvs = [ar.alloc([1024], F32) for _ in range(2)]
            Bkvs = [Buf(), Buf()]
            kr = [ar.alloc([256], F32) for _ in range(2)]
            Bkr = [Buf(), Buf()]
            for i in range(16):
                z = i % 2
                bk = ZB[rot["t"] % 2]
                rot["t"] += 1
                zproj(i, Wk, BWk, 320, bk)
                S.op("act", I("copy", out=zs[z][:, 0:320], in_=ps[bk][:, 0:320]), reads=[Bps[bk]], writes=[Bzs[z]])
                headnorm(zs[z][:, 0:256].rearrange("p (h d) -> p h d", h=1), Bzs[z], 1, 256, gckv, Bgc,
                         xb[z][:, 0:256].rearrange("p (h d) -> p h d", h=1), Bxb[z], sq_scr)
                transposes([xb[z][:, c * 128:(c + 1) * 128] for c in range(2)], Bxb[z],
                           lambda g0, n, z=z: ckT[z][:, g0:g0 + n, :], BckT[z])
                b0 = ZB[rot["t"] % 2]
                rot["t"] += 1
                b1 = ZB[rot["t"] % 2]
                rot["t"] += 1
                for half, bb in ((0, b0), (1, b1)):
                    for c in range(2):
                        mm(bb, ps[bb][:, :], ckT[z][:, c, :], Wukv[:, c, half * 512:(half + 1) * 512], c == 0, c == 1, [BckT[z], BWukv])
                    S.op("act", I("copy", out=kvs[z][:, half * 512:(half + 1) * 512], in_=ps[bb][:, :]),
                         reads=[Bps[bb]], writes=[Bkvs[z]])
                kv4 = kvs[z][:, :].rearrange("p (h t d) -> p h t d", h=4, t=2)
                S.op("dve", I("tensor_copy", out=Vx[:, i, :, 0:128], in_=kv4[:, :, 1, :]), reads=[Bkvs[z]], writes=[BV[i]])
                sqv = sq_scr[:, 0:512].rearrange("p (h d) -> p h d", h=4)
                Bsq = Buf()
                S.op("dve", I("tensor_tensor", out=sqv, in0=kv4[:, :, 0, :], in1=kv4[:, :, 0, :], op=ALU.mult),
                     reads=[Bkvs[z]], writes=[Bsq])
                ss = ar.alloc([8], F32)
                Bss = Buf()
                S.op("dve", I("tensor_reduce", out=ss[:, 0:4], in_=sqv, axis=AX.X, op=ALU.add), reads=[Bsq], writes=[Bss])
                S.op("act", I("activation", out=sq_scr[:, 512:576], in_=zs[z][:, 256:320], func=AF.Square, accum_out=ss[:, 4:5]),
                     reads=[Bzs[z]], writes=[Bss, Bsq])
                S.op("dve", I("tensor_scalar", out=ss[:, 0:4], in0=ss[:, 0:4], scalar1=ss[:, 4:5], scalar2=None, op0=ALU.add),
                     reads=[Bss], writes=[Bss])
                Brs = Buf()
                rs = rstd_of(ss[:, 0:4], 4, 192, [Bss], Brs)
                S.op("dve", I("tensor_tensor", out=sqv, in0=kv4[:, :, 0, :], in1=bcast(rs, [128, 4, 128]), op=ALU.mult),
                     reads=[Bkvs[z], Brs], writes=[Bsq])
                S.op("dve", I("tensor_tensor", out=xb[z][:, 0:512], in0=sq_scr[:, 0:512], in1=gkn, op=ALU.mult),
                     reads=[Bsq, Bgn], writes=[Bxb[z]])
                S.op("dve", I("tensor_tensor", out=sq_scr[:, 576:640], in0=zs[z][:, 256:320], in1=gkr, op=ALU.mult),
                     reads=[Bzs[z], Bgr], writes=[Bsq])
                rope(sq_scr[:, 576:640].rearrange("p (h d) -> p h d", h=1), Bsq, 1, i,
                     kr[z][:, 0:64].rearrange("p (h d) -> p h d", h=1), Bkr[z], sq_scr[:, 640:768])
                S.op("dve", I("tensor_tensor", out=xb[z][:, 512:768].rearrange("p (h d) -> p h d", h=4),
                                                                  in0=bc_mid(kr[z][:, 0:64], 4), in1=bcast(rs, [128, 4, 64]), op=ALU.mult),
                     reads=[Bkr[z], Brs], writes=[Bxb[z]])
                transposes([xb[z][:, h * 128:(h + 1) * 128] for h in range(4)], Bxb[z],
                           lambda g0, n, i=i: KTn[:, g0:g0 + n, i * 128:(i + 1) * 128], BKT[i])
                transposes([xb[z][:, 512 + p * 128:512 + (p + 1) * 128] for p in range(2)], Bxb[z],
                           lambda g0, n, i=i: KTr[:, g0:g0 + n, i * 128:(i + 1) * 128], BKT[i])
            S.barrier()
            ar.release(m1)
            PTb = [ar.alloc([4, 128], BF16) for _ in range(2)]
            BPT = [Buf(), Buf()]
            for j in range(8):
                def mla_scores(v, h, j=j):
                    lc = v
                    pr = slice((h % 2) * 64, (h % 2) * 64 + 64)
                    return [(KTn[:, h, lc * 128:(lc + 1) * 128], QTn[:, h, j * 128:(j + 1) * 128], [BKT[lc], BQT[j]]),
                            (KTr[pr, h // 2, lc * 128:(lc + 1) * 128], QTr[pr, h // 2, j * 128:(j + 1) * 128], [BKT[lc], BQT[j]])]
                attend(j, list(range(16)), 4, 128, mla_scores, 192 ** -0.5, lambda v: None, None,
                       lambda v, h: (Vx[:, v, h, 0:129], [BV[v]]), 130, 2, PTb, BPT,
                       finish_plain(j, 4, 128, 130, 2, 512))
            S.barrier()
            ar.release(m0)
            if stop_here("MLA"):
                return

            KTs = ar.alloc([4, 2048], BF16)
            BKT = [Buf() for _ in range(16)]
            Vs = ar.alloc([16, 2, 66], BF16)
            BV = [Buf() for _ in range(16)]
            QTs = ar.alloc([4, 1024], BF16)
            BQT = [Buf() for _ in range(8)]
            S.op("dve", I("memset", Vs[:, :, :, 64:66], 1.0), writes=BV)
            gsq = ar.alloc([512], F32)
            gsk = ar.alloc([128], F32)
            esink = ar.alloc([64], F32)
            Bgsq, Bgsk, Besk = Buf(), Buf(), Buf()
            bload(gsq, gv, "sq", Bgsq)
            bload(gsk, gv, "sk", Bgsk)
            bload(esink, gv, "sink", Besk)
            S.op("act", I("activation", out=esink, in_=esink, func=AF.Exp), reads=[Besk], writes=[Besk])
            Wk = ar.alloc([16, 256], BF16)
            BWk = Buf()
            wload(Wk, w_in[:, :, 3136:3392], BWk)
            Wq = ar.alloc([16, 512], BF16)
            BWq = Buf()
            wload(Wq, w_in[:, :, 2624:3136], BWq)
            zs, Bzs = zstage()
            sq_scr = ar.alloc([1024], F32)
            xb = [ar.alloc([1024], BF16) for _ in range(2)]
            Bxb = [Buf(), Buf()]
            xn = [ar.alloc([512], F32) for _ in range(2)]
            Bxn = [Buf(), Buf()]
            for i in range(16):
                z = i % 2
                bk = ZB[rot["t"] % 2]
                rot["t"] += 1
                zproj(i, Wk, BWk, 256, bk)
                S.op("act", I("copy", out=zs[z][:, 0:256], in_=ps[bk][:, 0:256]), reads=[Bps[bk]], writes=[Bzs[z]])
                S.op("dve", I("tensor_copy", out=Vs[:, i, :, 0:64], in_=zs[z][:, 128:256].rearrange("p (h d) -> p h d", h=2)),
                     reads=[Bzs[z]], writes=[BV[i]])
                k3 = xn[z][:, 0:128].rearrange("p (h d) -> p h d", h=2)
                headnorm(zs[z][:, 0:128].rearrange("p (h d) -> p h d", h=2), Bzs[z], 2, 64, gsk, Bgsk, k3, Bxn[z], sq_scr)
                if i < 2:
                    S.op("dve", I("memset", xb[z][:, 0:512], 0.0), writes=[Bxb[z]])
                x5 = xb[z][:, 0:512].rearrange("p (h v d) -> p h v d", h=2, v=2)
                rope(k3, Bxn[z], 2, i, x5[:, :, 0, 0:64], Bxb[z], sq_scr)
                S.op("dve", I("tensor_copy", out=x5[:, :, 1, 64:128], in_=x5[:, :, 0, 0:64]), reads=[Bxb[z]], writes=[Bxb[z]])
                transposes([xb[z][:, h * 128:(h + 1) * 128] for h in range(4)], Bxb[z],
                           lambda g0, n, i=i: KTs[:, g0:g0 + n, i * 128:(i + 1) * 128], BKT[i])
            for j in range(8):
                i = qoff + j
                z = j % 2
                bk = ZB[rot["t"] % 2]
                rot["t"] += 1
                zproj(i, Wq, BWq, 512, bk)
                S.op("act", I("copy", out=zs[z][:, 0:512], in_=ps[bk][:, :]), reads=[Bps[bk]], writes=[Bzs[z]])
                q3 = xn[z][:, 0:512].rearrange("p (h d) -> p h d", h=8)
                headnorm(zs[z][:, 0:512].rearrange("p (h d) -> p h d", h=8), Bzs[z], 8, 64, gsq, Bgsq, q3, Bxn[z], sq_scr)
                rope(q3, Bxn[z], 8, i, xb[z][:, 0:512].rearrange("p (h d) -> p h d", h=8), Bxb[z], sq_scr)
                transposes([xb[z][:, p * 128:(p + 1) * 128] for p in range(4)], Bxb[z],
                           lambda g0, n, j=j: QTs[:, g0:g0 + n, j * 128:(j + 1) * 128], BQT[j])
            PTb = [ar.alloc([4, 128], BF16) for _ in range(2)]
            BPT = [Buf(), Buf()]
            for j in range(8):
                i = qoff + j
                for hk in range(2):
                    visits = []
                    for hc in range(2):
                        J = (i + 8 * hc) % 16
                        for rel in (-1, 0, 1):
                            c = J + rel
                            if 0 <= c <= 15:
                                visits.append((hc, rel, (c + 8 * hc) % 16))

                    def sw_scores(v, g, j=j, hk=hk):
                        lc = v[2]
                        hq = hk * 4 + g
                        pr = slice((hq % 2) * 64, (hq % 2) * 64 + 64)
                        return [(KTs[:, hk * 2 + (hq % 2), lc * 128:(lc + 1) * 128], QTs[:, hq // 2, j * 128:(j + 1) * 128], [BKT[lc], BQT[j]])]

                    def sw_mask(v, PT, BP):
                        if v[1] == 0:
                            return
                        mi = 0 if v[1] == -1 else 1
                        S.op("dve", I("tensor_tensor", out=PT, in0=PT, in1=bc_mid(trim[:, mi, :], 4), op=ALU.mult),
                             reads=[BP, Bconst], writes=[BP])

                    def sw_finish(OB, j=j, hk=hk):
                        ob = OB[0]
                        rec = ar.alloc([4], F32)
                        Brec = Buf()
                        denv = ps[ob][:, 64:64 + 3 * 66 + 1:66]
                        S.op("dve", I("tensor_tensor", out=rec, in0=denv, in1=esink[:, hk * 4:hk * 4 + 4], op=ALU.add),
                             reads=[Bps[ob], Besk], writes=[Brec])
                        S.op("dve", I("reciprocal", out=rec, in_=rec), reads=[Brec], writes=[Brec])
                        for g in range(4):
                            hq = hk * 4 + g
                            S.op("dve", I("tensor_scalar",
                                out=br[:, j, 1024 + hq * 64:1024 + (hq + 1) * 64], in0=ps[ob][:, g * 66:g * 66 + 64],
                                scalar1=rec[:, g:g + 1], scalar2=None, op0=ALU.mult), reads=[Bps[ob], Brec], writes=[B_br[j]])

                    attend(j, visits, 4, 64, sw_scores, 64 ** -0.5, lambda v: lsel[:, v[0]:v[0] + 1], sw_mask,
                           lambda v, g, hk=hk: (Vs[:, v[2], hk, 0:65], [BV[v[2]]]), 66, 4, PTb, BPT, sw_finish)
            S.barrier()
            ar.release(m0)
            if stop_here("SWA"):
                return

            KTm = ar.alloc([4, 256], BF16)
            BKT = [Buf(), Buf()]
            Vm = ar.alloc([2, 4, 130], BF16)
            BV = [Buf(), Buf()]
            QTm = ar.alloc([4, 1024], BF16)
            BQT = [Buf() for _ in range(8)]
            S.op("dve", I("memset", Vm[:, :, :, 128:130], 1.0), writes=BV)
            memT = ar.alloc([16, 256], BF16)
            BmT = [Buf(), Buf()]
            gmq2 = ar.alloc([512], F32)
            gmk = ar.alloc([512], F32)
            Bgmn, Bgmq2, Bgmk = Buf(), Buf(), Buf()
            bload(gmq2, gv, "memq", Bgmq2)
            bload(gmk, gv, "memk", Bgmk)
            zs, Bzs = zstage(512)
            sq_scr = ar.alloc([512], F32)
            xb = [ar.alloc([512], BF16) for _ in range(2)]
            Bxb = [Buf(), Buf()]
            mM = ar.mark()
            gmn = ar.alloc([2048], F32)
            bload(gmn, gv, "memn", Bgmn)
            hl = [ar.alloc([2048], F32) for _ in range(2)]
            Bhl = [Buf(), Buf()]
            ub = [ar.alloc([2048], BF16) for _ in range(2)]
            Bub = [Buf(), Buf()]
            Wc = [ar.alloc([16, 512], BF16) for _ in range(2)]
            BWc = [Buf(), Buf()]
            mkv = W["mkv"].rearrange("(k p) n -> p k n", p=128)
            wload(Wc[0], mkv[:, :, 0:512], BWc[0])
            wload(Wc[1], mkv[:, :, 512:1024], BWc[1])
            for t in range(2):
                rmsnorm_tile(memd[t], [], gmn, Bgmn, hl[t], Bhl[t], ub[t], Bub[t])
                transposes([ub[t][:, k * 128:(k + 1) * 128] for k in range(16)], Bub[t],
                           lambda g0, n, t=t: memT[:, g0:g0 + n, t * 128:(t + 1) * 128], BmT[t])
                for half in range(2):
                    bk = ZB[rot["t"] % 2]
                    rot["t"] += 1
                    for k in range(16):
                        mm(bk, ps[bk][:, :], memT[:, k, t * 128:(t + 1) * 128], Wc[half][:, k, :], k == 0, k == 15, [BmT[t], BWc[half]])
                    if half == 1:
                        S.op("act", I("copy", out=Vm[:, t, :, 0:128], in_=ps[bk][:, :].rearrange("p (h d) -> p h d", h=4)),
                             reads=[Bps[bk]], writes=[BV[t]])
                    else:
                        S.op("act", I("copy", out=zs[t][:, 0:512], in_=ps[bk][:, :]), reads=[Bps[bk]], writes=[Bzs[t]])
                        headnorm(zs[t][:, 0:512].rearrange("p (h d) -> p h d", h=4), Bzs[t], 4, 128, gmk, Bgmk,
                                 xb[t][:, 0:512].rearrange("p (h d) -> p h d", h=4), Bxb[t], sq_scr)
                        transposes([xb[t][:, h * 128:(h + 1) * 128] for h in range(4)], Bxb[t],
                                   lambda g0, n, t=t: KTm[:, g0:g0 + n, t * 128:(t + 1) * 128], BKT[t])
            S.barrier()
            ar.release(mM)
            Wq = ar.alloc([16, 512], BF16)
            BWq = Buf()
            wload(Wq, w_in[:, :, 3392:3904], BWq)
            for j in range(8):
                i = qoff + j
                z = j % 2
                bk = ZB[rot["t"] % 2]
                rot["t"] += 1
                zproj(i, Wq, BWq, 512, bk)
                S.op("act", I("copy", out=zs[z][:, 0:512], in_=ps[bk][:, :]), reads=[Bps[bk]], writes=[Bzs[z]])
                headnorm(zs[z][:, 0:512].rearrange("p (h d) -> p h d", h=4), Bzs[z], 4, 128, gmq2, Bgmq2,
                         xb[z][:, 0:512].rearrange("p (h d) -> p h d", h=4), Bxb[z], sq_scr)
                transposes([xb[z][:, h * 128:(h + 1) * 128] for h in range(4)], Bxb[z],
                           lambda g0, n, j=j: QTm[:, g0:g0 + n, j * 128:(j + 1) * 128], BQT[j])
            PTb = [ar.alloc([4, 128], BF16) for _ in range(2)]
            BPT = [Buf(), Buf()]
            for j in range(8):
                def me_scores(v, h, j=j):
                    return [(KTm[:, h, v * 128:(v + 1) * 128], QTm[:, h, j * 128:(j + 1) * 128], [BKT[v], BQT[j]])]
                attend(j, [0, 1], 4, 128, me_scores, 128 ** -0.5, lambda v: None, None,
                       lambda v, h: (Vm[:, v, h, 0:129], [BV[v]]), 130, 2, PTb, BPT,
                       finish_plain(j, 4, 128, 130, 2, 1536))
            S.barrier()
            ar.release(base)

            if stop_here("MEM"):
                return

            brT = ar.alloc([16, 1024], BF16)
            BbrT = [Buf() for _ in range(8)]
            for j in range(8):
                transposes([br[:, j, c * 128:(c + 1) * 128] for c in range(16)], B_br[j],
                           lambda g0, n, j=j: brT[:, g0:g0 + n, j * 128:(j + 1) * 128], BbrT[j])
            S.barrier()
            mT = br.rearrange("p a b -> p (a b)").rearrange("p (a b) -> p a b", a=16)
            BmTt = [Buf() for _ in range(8)]
            acc = ar.alloc([4, 2, 512], F32)
            Bacc = [[Buf(), Buf()] for _ in range(4)]
            Wg = [ar.alloc([16, 512], BF16) for _ in range(2)]
            BWg = [Buf(), Buf()]
            Wb = [ar.alloc([4, 512], BF16) for _ in range(2)]
            BWb = [Buf(), Buf()]
            sgt = [ar.alloc([512], F32) for _ in range(2)]
            Bsg = [Buf(), Buf()]
            bgt = ar.alloc([64], F32)
            Bbg = Buf()
            S.dma("sp", I("dma_start", out=bgt, in_=W["bgT"]), writes=[Bbg])
            rb = [ar.alloc([512], F32) for _ in range(2)]
            Brb = [Buf(), Buf()]
            obf = sgt
            Bob = Bsg
            wgv = W["wg"].rearrange("(k p) n d -> p k n d", p=128)
            wbv = W["wbr"].rearrange("n (c p) d -> p n c d", p=128)
            cnt = 0
            for dblk in range(4):
                for n in range(4):
                    s = cnt % 2
                    cnt += 1
                    wload(Wg[s], wgv[:, :, n, dblk * 512:(dblk + 1) * 512], BWg[s])
                    wload(Wb[s], wbv[:, n, :, dblk * 512:(dblk + 1) * 512], BWb[s])
                    for dcl in range(4):
                        dc = dblk * 4 + dcl
                        for tb in range(2):
                            bp = rot["t"] % 2
                            bg = 2 + rot["t"] % 2
                            rot["t"] += 1
                            for c in range(4):
                                mm(bp, ps[bp][:, :], Wb[s][:, c, dcl * 128:(dcl + 1) * 128], brT[:, n * 4 + c, tb * 512:(tb + 1) * 512],
                                   c == 0, c == 3, [BWb[s]] + BbrT[tb * 4:tb * 4 + 4])
                            t0 = qoff * 128 + tb * 512
                            for k in range(16):
                                mm(bg, ps[bg][:, :], Wg[s][:, k, dcl * 128:(dcl + 1) * 128], uT[:, k, t0:t0 + 512],
                                   k == 0, k == 15, [BWg[s]] + B_uT[qoff + tb * 4:qoff + tb * 4 + 4])
                            sg = sgt[tb]
                            S.op("act", I("activation", out=sg, in_=ps[bg][:, :], func=AF.Sigmoid,
                                                                                         bias=bgt[:, n * 16 + dc:n * 16 + dc + 1], scale=1.0),
                                 reads=[Bps[bg], Bbg], writes=[Bsg[tb]])
                            if n == 0:
                                S.op("dve", I("tensor_tensor", out=acc[:, dcl, tb, :], in0=sg, in1=ps[bp][:, :], op=ALU.mult),
                                     reads=[Bsg[tb], Bps[bp]], writes=[Bacc[dcl][tb]])
                            else:
                                S.op("dve", I("tensor_tensor", out=sg, in0=sg, in1=ps[bp][:, :], op=ALU.mult),
                                     reads=[Bsg[tb], Bps[bp]], writes=[Bsg[tb]])
                                if n < 3:
                                    S.op("dve", I("tensor_tensor", out=acc[:, dcl, tb, :], in0=acc[:, dcl, tb, :], in1=sg, op=ALU.add),
                                         reads=[Bsg[tb], Bacc[dcl][tb]], writes=[Bacc[dcl][tb]])
                                else:
                                    S.op("dve", I("tensor_tensor", out=mT[:, dc, tb * 512:(tb + 1) * 512], in0=acc[:, dcl, tb, :], in1=sg, op=ALU.add),
                                         reads=[Bsg[tb], Bacc[dcl][tb]], writes=BmTt[tb * 4:tb * 4 + 4])
            wov = W["wo"].rearrange("(k p) n -> p k n", p=128)
            for cb in range(4):
                s = cnt % 2
                cnt += 1
                wload(Wg[s], wov[:, :, cb * 512:(cb + 1) * 512], BWg[s])
                for j in range(8):
                    bk = rot["t"] % 4
                    rot["t"] += 1
                    for dc in range(16):
                        mm(bk, ps[bk][:, :], mT[:, dc, j * 128:(j + 1) * 128], Wg[s][:, dc, :], dc == 0, dc == 15, [BmTt[j], BWg[s]])
                    r = (j + cb) % 2
                    S.dma("sp", I("dma_start", out=rb[r], in_=src[qoff + j][:, cb * 512:(cb + 1) * 512]),
                          reads=[Bsrc[qoff + j][cb]], writes=[Brb[r]])
                    S.op("dve", I("tensor_tensor", out=obf[r], in0=ps[bk][:, :], in1=rb[r], op=ALU.add),
                         reads=[Bps[bk], Brb[r]], writes=[Bob[r]])
                    S.dma("sp", I("dma_start", out=hmid[j][:, cb * 512:(cb + 1) * 512], in_=obf[r]),
                          reads=[Bob[r]], writes=[Bd["hmid"][j][cb]])
            S.barrier()
            ar.release(0)
            if stop_here("MERGE"):
                return

            dst = dram[dst_name]
            dtile0 = qoff if dst_name == "h1s" else 0
            Bdst = Bd[dst_name]
            hnT = ar.alloc([16, 1024], BF16)
            BhnT = [Buf() for _ in range(8)]
            comb = ar.alloc([8, 8], F32)
            Bcomb = [Buf() for _ in range(8)]
            m2 = ar.mark()
            gff = ar.alloc([2048], F32)
            Bgff = Buf()
            bload(gff, gv, "ffn", Bgff)
            hl = [ar.alloc([2048], F32) for _ in range(2)]
            Bhl = [Buf(), Buf()]
            ub = [ar.alloc([2048], BF16) for _ in range(2)]
            Bub = [Buf(), Buf()]
            moe = (l % 2 == 1)
            if moe:
                wr = ar.alloc([16, 8], F32)
                Bwr = Buf()
                S.dma("sp", I("dma_start", out=wr, in_=W["router"].rearrange("(k p) n -> p k n", p=128)), writes=[Bwr])
                hn32 = [ar.alloc([2048], F32) for _ in range(2)]
                Bhn32 = [Buf(), Buf()]
                hT32 = [ar.alloc([16, 128], F32) for _ in range(2)]
                BhT32 = [Buf(), Buf()]
            for j in range(8):
                s = j % 2
                rs, Brs = rmsnorm_tile(hmid[j], Bd["hmid"][j], gff, Bgff, hl[s], Bhl[s], ub[s], Bub[s])
                transposes([ub[s][:, k * 128:(k + 1) * 128] for k in range(16)], Bub[s],
                           lambda g0, n, j=j: hnT[:, g0:g0 + n, j * 128:(j + 1) * 128], BhnT[j])
                if moe:
                    S.op("dve", I("scalar_tensor_tensor", out=hn32[s], in0=hl[s], scalar=rs[:, 0:1], in1=gff, op0=ALU.mult, op1=ALU.mult),
                         reads=[Bhl[s], Brs, Bgff], writes=[Bhn32[s]])
                    for g in range(4):
                        bk = rot["t"] % 2
                        rot["t"] += 1
                        for q in range(4):
                            k = g * 4 + q
                            S.op("pe", I("transpose", out=ps[bk][:, q * 128:(q + 1) * 128], in_=hn32[s][:, k * 128:(k + 1) * 128], identity=identf[:]),
                                 reads=[Bhn32[s], Bconst], writes=[Bps[bk]])
                        S.op("act", I("copy", out=hT32[s][:, g * 4:(g + 1) * 4, :], in_=ps[bk][:, :].rearrange("p (n t) -> p n t", n=4)),
                             reads=[Bps[bk]], writes=[BhT32[s]])
                    bk = 2 + rot["t"] % 2
                    rot["t"] += 1
                    for k in range(16):
                        mm(bk, ps[bk][:, 0:8], hT32[s][:, k, :], wr[:, k, :], k == 0, k == 15, [BhT32[s], Bwr])
                    lg = ar.alloc([8], F32)
                    e1 = ar.alloc([8], F32)
                    l2 = ar.alloc([8], F32)
                    e2 = ar.alloc([8], F32)
                    mx = ar.alloc([4], F32)
                    Bt = Buf()
                    S.op("dve", I("tensor_copy", out=lg, in_=ps[bk][:, 0:8]), reads=[Bps[bk]], writes=[Bt])
                    S.op("dve", I("tensor_reduce", out=mx[:, 0:1], in_=lg, axis=AX.X, op=ALU.max), reads=[Bt], writes=[Bt])
                    S.op("dve", I("tensor_scalar", out=e1, in0=lg, scalar1=mx[:, 0:1], scalar2=None, op0=ALU.is_equal), reads=[Bt], writes=[Bt])
                    S.op("dve", I("scalar_tensor_tensor", out=l2, in0=e1, scalar=-1e30, in1=lg, op0=ALU.mult, op1=ALU.add), reads=[Bt], writes=[Bt])
                    S.op("dve", I("tensor_reduce", out=mx[:, 1:2], in_=l2, axis=AX.X, op=ALU.max), reads=[Bt], writes=[Bt])
                    S.op("dve", I("tensor_scalar", out=e2, in0=l2, scalar1=mx[:, 1:2], scalar2=None, op0=ALU.is_equal), reads=[Bt], writes=[Bt])
                    S.op("dve", I("tensor_tensor", out=mx[:, 2:3], in0=mx[:, 0:1], in1=mx[:, 1:2], op=ALU.subtract), reads=[Bt], writes=[Bt])
                    S.op("act", I("activation", out=mx[:, 3:4], in_=mx[:, 2:3], func=AF.Sigmoid, scale=-1.0), reads=[Bt], writes=[Bt])
                    S.op("act", I("activation", out=mx[:, 2:3], in_=mx[:, 2:3], func=AF.Sigmoid), reads=[Bt], writes=[Bt])
                    S.op("dve", I("tensor_scalar", out=e1, in0=e1, scalar1=mx[:, 2:3], scalar2=None, op0=ALU.mult), reads=[Bt], writes=[Bt])
                    S.op("dve", I("scalar_tensor_tensor", out=comb[:, j, :], in0=e2, scalar=mx[:, 3:4], in1=e1, op0=ALU.mult, op1=ALU.add),
                         reads=[Bt], writes=[Bcomb[j]])
            S.barrier()
            ar.release(m2)

            units = []
            if not moe:
                upv = W["up"].rearrange("(k p) n -> p k n", p=128)
                dnv = W["down"].rearrange("(f p) n -> p f n", p=128)
                for hh in range(2):
                    units.append((upv[:, :, hh * 2816:(hh + 1) * 2816], upv[:, :, 5632 + hh * 2816:5632 + (hh + 1) * 2816],
                                  dnv[:, hh * 22:(hh + 1) * 22, :], 22, None))
            else:
                for ex in range(8):
                    upv = W["mup"][ex].rearrange("(k p) n -> p k n", p=128)
                    dnv = W["mdown"][ex].rearrange("(f p) n -> p f n", p=128)
                    for hh in range(2):
                        units.append((upv[:, :, hh * 3584:(hh + 1) * 3584], upv[:, :, 7168 + hh * 3584:7168 + (hh + 1) * 3584],
                                      dnv[:, hh * 28:(hh + 1) * 28, :], 28, ex))
            FcM = units[0][3]
            actT = ar.alloc([FcM, 1024], BF16)
            Bact = [Buf() for _ in range(FcM)]
            Wu = [ar.alloc([2, 16, 256], BF16) for _ in range(2)]
            BWu = [[Buf(), Buf()], [Buf(), Buf()]]
            Wdn = [ar.alloc([FcM, 512], BF16) for _ in range(2)]
            BWdn = [Buf(), Buf()]
            sgt = [ar.alloc([512], F32) for _ in range(2)]
            Bsg = [Buf(), Buf()]
            rb = [ar.alloc([512], F32) for _ in range(2)]
            Brb = [Buf(), Buf()]
            obf = [ar.alloc([512], F32) for _ in range(2)]
            Bob = [Buf(), Buf()]
            cu = 0
            cd = 0
            for ui, (ug, uu, dn, Fc, ex) in enumerate(units):
                for fg in range(Fc // 2):
                    s = cu % 2
                    cu += 1
                    S.dma("pool", I("dma_start", out=Wu[s][:, 0, :, :], in_=ug[:, :, fg * 256:(fg + 1) * 256]), writes=[BWu[s][0]])
                    S.dma("pool", I("dma_start", out=Wu[s][:, 1, :, :], in_=uu[:, :, fg * 256:(fg + 1) * 256]), writes=[BWu[s][1]])
                    for fl in range(2):
                        f = fg * 2 + fl
                        for tb in range(2):
                            bgk = rot["t"] % 2
                            buk = 2 + rot["t"] % 2
                            rot["t"] += 1
                            for k in range(16):
                                mm(bgk, ps[bgk][:, :], Wu[s][:, 0, k, fl * 128:(fl + 1) * 128], hnT[:, k, tb * 512:(tb + 1) * 512],
                                   k == 0, k == 15, [BWu[s][0]] + BhnT[tb * 4:tb * 4 + 4])
                            for k in range(16):
                                mm(buk, ps[buk][:, :], Wu[s][:, 1, k, fl * 128:(fl + 1) * 128], hnT[:, k, tb * 512:(tb + 1) * 512],
                                   k == 0, k == 15, [BWu[s][1]] + BhnT[tb * 4:tb * 4 + 4])
                            sg = sgt[tb]
                            S.op("act", I("activation", out=sg, in_=ps[bgk][:, :], func=AF.Silu), reads=[Bps[bgk]], writes=[Bsg[tb]])
                            S.op("dve", I("tensor_tensor", out=actT[:, f, tb * 512:(tb + 1) * 512], in0=sg, in1=ps[buk][:, :], op=ALU.mult),
                                 reads=[Bsg[tb], Bps[buk]], writes=[Bact[f]])
                last = (ui == len(units) - 1)
                rsrc, Brsrc = (hmid, Bd["hmid"]) if ui == 0 else (hacc, Bd["hacc"])
                for cb in range(4):
                    s = cd % 2
                    cd += 1
                    S.dma("pool", I("dma_start", out=Wdn[s][:, 0:Fc, :], in_=dn[:, :, cb * 512:(cb + 1) * 512]), writes=[BWdn[s]])
                    for j in range(8):
                        bk = 4 + rot["t"] % 4
                        rot["t"] += 1
                        for f in range(Fc):
                            mm(bk, ps[bk][:, :], actT[:, f, j * 128:(j + 1) * 128], Wdn[s][:, f, :], f == 0, f == Fc - 1, [Bact[f], BWdn[s]])
                        r = (j + cb) % 2
                        S.dma("sp", I("dma_start", out=rb[r], in_=rsrc[j][:, cb * 512:(cb + 1) * 512]),
                              reads=[Brsrc[j][cb]], writes=[Brb[r]])
                        if ex is None:
                            S.op("dve", I("tensor_tensor", out=obf[r], in0=ps[bk][:, :], in1=rb[r], op=ALU.add),
                                 reads=[Bps[bk], Brb[r]], writes=[Bob[r]])
                        else:
                            S.op("dve", I("scalar_tensor_tensor", out=obf[r], in0=ps[bk][:, :], scalar=comb[:, j, ex:ex + 1], in1=rb[r],
                                                                                                 op0=ALU.mult, op1=ALU.add),
                                 reads=[Bps[bk], Brb[r], Bcomb[j]], writes=[Bob[r]])
                        if last:
                            S.dma("sp", I("dma_start", out=dst[dtile0 + j][:, cb * 512:(cb + 1) * 512], in_=obf[r]),
                                  reads=[Bob[r]], writes=[Bdst[dtile0 + j][cb]])
                        else:
                            S.dma("sp", I("dma_start", out=hacc[j][:, cb * 512:(cb + 1) * 512], in_=obf[r]),
                                  reads=[Bob[r]], writes=[Bd["hacc"][j][cb]])
            S.barrier()

        for (l, qoff, sn, dn_) in passes:
            layer_pass(l, qoff, sn, dn_)
        S.barrier()
        S.emit()
    return nc


def _consts(half):
    pos = np.zeros((128, 16), np.int64)
    for i in range(16):
        J = (i + 8 * half) % 16
        pos[:, i] = J * 128 + np.arange(128)
    freqs = (np.float32(10000.0) ** (np.float32(-2.0) * np.arange(32, dtype=np.float32) / np.float32(64))).astype(np.float32)
    ang = pos.astype(np.float32)[:, :, None] * freqs[None, None, :]
    cs = np.concatenate([np.cos(ang), np.sin(ang)], axis=-1).astype(np.float32)
    lsel = np.full((128, 2), NEG, np.float32)
    lsel[:, half] = 0.0
    ident = np.eye(128, dtype=np.float32)
    anti = np.eye(64, dtype=np.float32)[::-1].copy()
    qc = np.arange(64)
    cstart = np.clip(qc - 8, 0, 48)
    kc = np.arange(64)
    cm = ((kc[:, None] >= cstart[None, :]) & (kc[:, None] <= cstart[None, :] + 15)).astype(np.float32)
    colmask = np.concatenate([cm, cm], axis=0)
    k = np.arange(128)[:, None]
    q = np.arange(128)[None, :]
    trimask = np.stack([(k >= q), (k <= q)], axis=1).astype(np.float32)
    return dict(cs=cs, lsel=lsel, ident=ident, antiI=anti, colmask=colmask, trimask=trimask)


def _tile(v, n):
    return np.tile(np.asarray(v, np.float32), n)


def _layer_inputs(l, inp):
    d = {}
    d["w_in%d" % l] = inp["w_in"][l]
    d["uq%d" % l] = inp["mla_w_uq"][l]
    d["ukv%d" % l] = inp["mla_w_ukv"][l]
    d["mkv%d" % l] = inp["mem_w_kv"][l]
    d["wbr%d" % l] = inp["w_branch"][l]
    d["wg%d" % l] = inp["w_gate"][l]
    d["wo%d" % l] = inp["w_o"][l]
    mk = np.asarray(inp["mla_k_norm"][l], np.float32)
    gv = np.concatenate([
        inp["norm_mix"][l], _tile(inp["na_q_norm"][l], 4), _tile(inp["na_k_norm"][l], 4), inp["mla_cq_norm"][l],
        inp["mla_ckv_norm"][l], _tile(inp["mla_q_norm"][l], 4), _tile(mk[:128], 4), mk[128:],
        _tile(inp["swa_q_norm"][l], 8), _tile(inp["swa_k_norm"][l], 2), inp["mem_norm"][l],
        _tile(inp["mem_q_norm"][l], 4), _tile(inp["mem_k_norm"][l], 4), inp["norm_ffn"][l], inp["swa_sink"][l],
        np.zeros(56, np.float32)]).astype(np.float32)
    assert gv.shape[0] == NGV
    d["gv%d" % l] = gv[None, :]
    d["bgT%d" % l] = np.ascontiguousarray(np.asarray(inp["b_gate"][l], np.float32).reshape(4, 16, 128).transpose(2, 0, 1).reshape(128, 64))
    rp = np.zeros((60, 160), np.float32)
    r = np.asarray(inp["na_rpb"][l], np.float32)[:, ::-1, :].reshape(60, 31)
    rp[:, 48:48 + 31] = r
    d["rpbp%d" % l] = rp
    if l % 2 == 0:
        d["ffn_up"] = inp["ffn_w_up"][l // 2]
        d["ffn_down"] = inp["ffn_w_down"][l // 2]
    else:
        d["router"] = inp["moe_router"][l // 2]
        d["moe_up"] = inp["moe_w_up"][l // 2]
        d["moe_down"] = inp["moe_w_down"][l // 2]
    return d


def _perm_rows(hb, half):
    t = hb.reshape(16, 128, D)
    idx = [(i + 8 * half) % 16 for i in range(16)]
    return np.ascontiguousarray(t[idx])


_CACHE = {}


def _get_prog(key, passes, layers):
    if key not in _CACHE:
        _CACHE[key] = build(passes, layers, True)
    return _CACHE[key]


def _launch(nc, h_full, inp, layers):
    in_maps = []
    for c in range(8):
        b, half = c // 2, c % 2
        m = {"xin": _perm_rows(h_full[b], half), "mem": np.ascontiguousarray(np.asarray(inp["mem"][b], np.float32).reshape(2, 128, D))}
        m.update(_consts(half))
        for l in layers:
            m.update(_layer_inputs(l, inp))
        in_maps.append(m)
    names = set()
    for alloc in nc.allocations:
        if isinstance(alloc, mybir.MemoryLocationSet) and alloc.kind == "ExternalInput":
            names.add(alloc.memorylocations[0].name)
    in_maps = [{k: v for k, v in m.items() if k in names} for m in in_maps]
    res = run_bass_kernel_spmd(nc, in_maps, core_ids=list(range(8)))
    out = np.zeros((4, 2048, D), np.float32)
    for c in range(8):
        b, half = c // 2, c % 2
        out[b, half * 1024:(half + 1) * 1024] = res.results[c]["hout"].reshape(1024, D)
    return out, res


def kernel(**inp):
    inp = {k: np.asarray(v) for k, v in inp.items()}
    x = np.asarray(inp["x"], np.float32)
    if FUSED:
        nc = _get_prog("fused", [(0, 0, "xin", "h1s"), (0, 8, "xin", "h1s"), (1, 0, "h1s", "hout")], [0, 1])
        out, _ = _launch(nc, x, inp, [0, 1])
        return out
    nc0 = _get_prog("l0", [(0, 0, "xin", "hout")], [0])
    h1, _ = _launch(nc0, x, inp, [0])
    nc1 = _get_prog("l1", [(1, 0, "xin", "hout")], [1])
    out, _ = _launch(nc1, h1, inp, [1])
    return out
```
